# Optimizing a Trainium2 kernel written in Bass

```python
import math
import jax, jax.numpy as jnp
from jax import lax
import numpy as np

D_MODEL = 1024
BATCH = 8
SEQ = 4096
DEPTH = 4

N_MIXERS = 3
N_RET = len(range(0, DEPTH, N_MIXERS))
N_CF = len(range(1, DEPTH, N_MIXERS))
N_SC = len(range(2, DEPTH, N_MIXERS))
RET_HEADS = 4
RET_DK = D_MODEL // RET_HEADS
RET_VDIM = 2 * D_MODEL
RET_DV = RET_VDIM // RET_HEADS
RET_CHUNK = 128
RET_IN = 2 * D_MODEL + 2 * RET_VDIM
ROPE_BASE = 10000.0
CF_KERNEL = 31
SC_KERNEL = 3
D_FF = 4 * D_MODEL
RMS_EPS = 1e-6
LN_EPS = 1e-5

kernel_name = "hybrid_retention_conformer_shortconv_trunk"

F32 = jnp.float32


def rms_norm(x, g):
    xf = x.astype(F32)
    y = xf * lax.rsqrt(jnp.mean(xf * xf, axis=-1, keepdims=True) + RMS_EPS)
    return (y * g.astype(F32)).astype(x.dtype)


def causal_depthwise_conv(x, w):
    k = w.shape[0]
    return lax.conv_general_dilated(
        x, w[:, None, :].astype(x.dtype), window_strides=(1,),
        padding=[(k - 1, 0)], dimension_numbers=("NWC", "WIO", "NWC"),
        feature_group_count=x.shape[-1])


def apply_rotary(t, cos, sin):
    t1, t2 = jnp.split(t, 2, axis=-1)
    return jnp.concatenate([t1 * cos - t2 * sin, t2 * cos + t1 * sin], axis=-1)


def chunkwise_retention(q, k, v):
    bsz, s, h, dk = q.shape
    dv = v.shape[-1]
    nc = s // RET_CHUNK
    log_gamma = jnp.log1p(-jnp.exp2(-5.0 - jnp.arange(h, dtype=F32)))
    idx = jnp.arange(RET_CHUNK, dtype=F32)
    rel = idx[:, None] - idx[None, :]
    intra = jnp.where(rel >= 0, jnp.exp(log_gamma[:, None, None] * jnp.maximum(rel, 0.0)), 0.0)
    q_decay = jnp.exp(log_gamma[:, None] * (idx + 1.0))
    k_decay = jnp.exp(log_gamma[:, None] * (RET_CHUNK - 1.0 - idx))
    chunk_decay = jnp.exp(log_gamma * RET_CHUNK)

    def to_chunks(t):
        return t.reshape(bsz, nc, RET_CHUNK, h, t.shape[-1]).transpose(1, 0, 3, 2, 4)

    def step(state, qkv):
        qc, kc, vc = qkv
        scores = jnp.einsum("bhid,bhjd->bhij", qc, kc) * intra
        out = (jnp.einsum("bhij,bhjv->bhiv", scores, vc)
               + jnp.einsum("bhid,bhdv->bhiv", qc * q_decay[..., None], state))
        state = (state * chunk_decay[:, None, None]
                 + jnp.einsum("bhjd,bhjv->bhdv", kc * k_decay[..., None], vc))
        return state, out

    state0 = jnp.zeros((bsz, h, dk, dv), F32)
    _, ys = lax.scan(step, state0, (to_chunks(q), to_chunks(k), to_chunks(v)))
    return ys.transpose(1, 0, 3, 2, 4).reshape(bsz, s, h, dv)


def retention_mixer(x, positions, w_in, gn_g, w_out):
    bsz, s, _ = x.shape
    proj = x @ w_in
    q, k, v, g = jnp.split(proj, [D_MODEL, 2 * D_MODEL, 2 * D_MODEL + RET_VDIM], axis=-1)
    q = q.reshape(bsz, s, RET_HEADS, RET_DK).astype(F32)
    k = k.reshape(bsz, s, RET_HEADS, RET_DK).astype(F32)
    v = v.reshape(bsz, s, RET_HEADS, RET_DV).astype(F32)
    inv_freq = ROPE_BASE ** (-jnp.arange(0, RET_DK, 2, dtype=F32) / RET_DK)
    ang = positions.astype(F32)[..., None, None] * inv_freq
    cos, sin = jnp.cos(ang), jnp.sin(ang)
    q = apply_rotary(q, cos, sin)
    k = apply_rotary(k, cos, sin) * (RET_DK ** -0.5)
    y = chunkwise_retention(q, k, v)
    mean = jnp.mean(y, axis=-1, keepdims=True)
    var = jnp.mean(jnp.square(y - mean), axis=-1, keepdims=True)
    y = ((y - mean) * lax.rsqrt(var + LN_EPS)).reshape(bsz, s, RET_VDIM) * gn_g.astype(F32)
    y = (jax.nn.silu(g.astype(F32)) * y).astype(x.dtype)
    return y @ w_out


def conformer_conv_mixer(x, w_pw1, b_pw1, w_dw, b_dw, ln_g, ln_b, w_pw2, b_pw2):
    h = x @ w_pw1 + b_pw1
    a, gate = jnp.split(h, 2, axis=-1)
    h = a * jax.nn.sigmoid(gate)
    h = causal_depthwise_conv(h, w_dw) + b_dw
    hf = h.astype(F32)
    mean = jnp.mean(hf, axis=-1, keepdims=True)
    var = jnp.mean(jnp.square(hf - mean), axis=-1, keepdims=True)
    hf = (hf - mean) * lax.rsqrt(var + LN_EPS) * ln_g.astype(F32) + ln_b.astype(F32)
    h = jax.nn.silu(hf).astype(x.dtype)
    return h @ w_pw2 + b_pw2


def short_conv_mixer(x, w_in, w_conv, w_out):
    gb, gc, h = jnp.split(x @ w_in, 3, axis=-1)
    u = causal_depthwise_conv(gc * h, w_conv)
    return (gb * u) @ w_out


def squared_relu_mlp(x, w1, w2):
    return jnp.square(jax.nn.relu(x @ w1)) @ w2


def setup_inputs(seed: int = 0) -> dict:
    key = jax.random.key(seed)
    ks = iter(jax.random.split(key, 32))

    def dense(shape, fan_in):
        return jax.random.normal(next(ks), shape, F32) * (fan_in ** -0.5)

    def small(shape, scale):
        return jax.random.normal(next(ks), shape, F32) * scale

    x = jax.random.normal(next(ks), (BATCH, SEQ, D_MODEL), F32)
    positions = jnp.broadcast_to(jnp.arange(SEQ, dtype=jnp.int32), (BATCH, SEQ))
    return {
        "x": x,
        "positions": positions,
        "norm_g": 1.0 + small((DEPTH, 4, D_MODEL), 0.05),
        "ret_w_in": dense((N_RET, D_MODEL, RET_IN), D_MODEL),
        "ret_gn_g": 1.0 + small((N_RET, RET_VDIM), 0.05),
        "ret_w_out": dense((N_RET, RET_VDIM, D_MODEL), RET_VDIM),
        "cf_w_pw1": dense((N_CF, D_MODEL, 2 * D_MODEL), D_MODEL),
        "cf_b_pw1": small((N_CF, 2 * D_MODEL), 0.01),
        "cf_w_dw": dense((N_CF, CF_KERNEL, D_MODEL), CF_KERNEL),
        "cf_b_dw": small((N_CF, D_MODEL), 0.01),
        "cf_ln_g": 1.0 + small((N_CF, D_MODEL), 0.05),
        "cf_ln_b": small((N_CF, D_MODEL), 0.01),
        "cf_w_pw2": dense((N_CF, D_MODEL, D_MODEL), D_MODEL),
        "cf_b_pw2": small((N_CF, D_MODEL), 0.01),
        "sc_w_in": dense((N_SC, D_MODEL, 3 * D_MODEL), D_MODEL),
        "sc_w_conv": dense((N_SC, SC_KERNEL, D_MODEL), SC_KERNEL),
        "sc_w_out": dense((N_SC, D_MODEL, D_MODEL), D_MODEL),
        "mlp_w1": dense((DEPTH, D_MODEL, D_FF), D_MODEL),
        "mlp_w2": dense((DEPTH, D_FF, D_MODEL), D_FF),
    }


def reference(x, positions, norm_g, ret_w_in, ret_gn_g, ret_w_out,
              cf_w_pw1, cf_b_pw1, cf_w_dw, cf_b_dw, cf_ln_g, cf_ln_b, cf_w_pw2, cf_b_pw2,
              sc_w_in, sc_w_conv, sc_w_out, mlp_w1, mlp_w2):
    for i in range(DEPTH):
        kind, j = i % N_MIXERS, i // N_MIXERS
        h = rms_norm(x, norm_g[i, 0])
        if kind == 0:
            h = retention_mixer(h, positions, ret_w_in[j], ret_gn_g[j], ret_w_out[j])
        elif kind == 1:
            h = conformer_conv_mixer(h, cf_w_pw1[j], cf_b_pw1[j], cf_w_dw[j], cf_b_dw[j],
                                     cf_ln_g[j], cf_ln_b[j], cf_w_pw2[j], cf_b_pw2[j])
        else:
            h = short_conv_mixer(h, sc_w_in[j], sc_w_conv[j], sc_w_out[j])
        x = x + rms_norm(h, norm_g[i, 1])
        h = squared_relu_mlp(rms_norm(x, norm_g[i, 2]), mlp_w1[i], mlp_w2[i])
        x = x + rms_norm(h, norm_g[i, 3])
    return x
```

```python
import math
from contextlib import ExitStack

import ml_dtypes
import numpy as np

import concourse.bass as bass
import concourse.mybir as mybir
from concourse.bass_utils import run_bass_kernel_spmd

F32 = mybir.dt.float32
BF16 = mybir.dt.bfloat16
I32 = mybir.dt.int32
AF = mybir.ActivationFunctionType
ALU = mybir.AluOpType

D = 1024
S = 4096
T = 512
NCH = 8
DEPTH = 4
NSLOT = 3
RMS_EPS = 1e-6
LN_EPS = 1e-5
TWO_PI = 2.0 * math.pi
CW1 = 6.28125
CW2 = TWO_PI - CW1

ENGS = ("pe", "act", "dve", "pool", "sp")
SEM_LIMIT = 30000


class Op:
    __slots__ = ("eng", "fn", "waits", "signal", "cseq", "comp", "vc", "ndma", "idx", "sigcount")


class Tracker:
    def __init__(self):
        self.ops = []
        self.eng_ops = {e: [] for e in ENGS}
        self.eng_clock = {e: {} for e in ENGS}
        self.last_w = {}
        self.readers = {}
        self.dma_count = {}

    def add(self, eng, fn, reads=(), writes=(), dma=None, ndma=1):
        op = Op()
        op.eng = eng
        op.fn = fn
        op.signal = False
        op.ndma = ndma
        op.idx = len(self.ops)
        elist = self.eng_ops[eng]
        if dma is None:
            op.comp = eng
            op.cseq = len(elist) + 1
        else:
            op.comp = ("dma", dma)
            self.dma_count[dma] = self.dma_count.get(dma, 0) + ndma
            op.cseq = self.dma_count[dma]
        deps = {}
        for k in reads:
            w = self.last_w.get(k)
            if w is not None:
                deps[w.idx] = (w, True)
        for k in writes:
            w = self.last_w.get(k)
            if w is not None and w.idx not in deps:
                deps[w.idx] = (w, False)
            for r in self.readers.get(k, ()):
                if r.idx not in deps:
                    deps[r.idx] = (r, False)
        clock = dict(self.eng_clock[eng])
        waits = []
        ncur = len(elist) + 1
        for di in sorted(deps):
            d, raw = deps[di]
            src, seq = d.comp, d.cseq
            if clock.get(src, 0) >= seq:
                continue
            if src == eng:
                if eng == "pe" or not raw:
                    continue
            waits.append(d)
            d.signal = True
            for s2, q2 in d.vc.items():
                if clock.get(s2, 0) < q2:
                    clock[s2] = q2
        op.waits = waits
        self.eng_clock[eng] = clock
        vc = dict(clock)
        vc[op.comp] = op.cseq
        op.vc = vc
        for k in reads:
            self.readers.setdefault(k, []).append(op)
        for k in writes:
            self.last_w[k] = op
            self.readers[k] = []
        self.ops.append(op)
        elist.append(op)
        return op

    def emit(self, nc, stack, final_waits=()):
        nsig = {}
        for e in ENGS:
            c = 0
            for op in self.eng_ops[e]:
                if op.comp == e and op.signal:
                    c += 1
                    op.sigcount = c
            nsig[e] = c
        sems = {}
        for e in ENGS:
            nep = (nsig[e] + SEM_LIMIT - 1) // SEM_LIMIT
            sems[e] = [stack.enter_context(nc.semaphore(f"s_{e}_{i}")) for i in range(max(nep, 1))]
        dsems = {}
        for name in self.dma_count:
            dsems[name] = stack.enter_context(nc.semaphore(f"d_{name}"))

        def semval(d):
            if isinstance(d.comp, tuple):
                return dsems[d.comp[1]], 16 * d.cseq
            c = d.sigcount
            return sems[d.comp][(c - 1) // SEM_LIMIT], (c - 1) % SEM_LIMIT + 1

        block = stack.enter_context(nc.Block())
        tr = self

        def run(engname):
            def body(eng):
                for op in tr.eng_ops[engname]:
                    for d in op.waits:
                        s, v = semval(d)
                        eng.wait_ge(s, v)
                    if isinstance(op.comp, tuple):
                        op.fn(eng, dsems[op.comp[1]])
                    else:
                        ins = op.fn(eng, None)
                        if op.signal:
                            s, v = semval(op)
                            ins.then_inc(s, 1)
                if engname == "pool":
                    for d in final_waits:
                        s, v = semval(d)
                        eng.wait_ge(s, v)
            return body

        block.tensor(run("pe"))
        block.scalar(run("act"))
        block.vector(run("dve"))
        block.gpsimd(run("pool"))
        block.sync(run("sp"))
        return nsig


def layer_kind(L):
    return L % 3, L // 3


def weight_pieces():
    pieces = []
    index = {}

    def addp(key, name, j, kc0, nkc, c0, ncols):
        index[key] = len(pieces)
        pieces.append((name, j, kc0, nkc, c0, ncols))

    for L in range(DEPTH):
        kind, j = layer_kind(L)
        if kind == 0:
            for h in range(4):
                addp((L, "q", h), "ret_w_in", j, 0, 8, h * 256, 256)
                addp((L, "k", h), "ret_w_in", j, 0, 8, 1024 + h * 256, 256)
                addp((L, "v", h), "ret_w_in", j, 0, 8, 2048 + h * 512, 512)
                addp((L, "g", h), "ret_w_in", j, 0, 8, 4096 + h * 512, 512)
            for p in range(4):
                addp((L, "o", p), "ret_w_out", j, 0, 16, p * 256, 256)
        elif kind == 1:
            for cg in range(2):
                addp((L, "a", cg), "cf_w_pw1", j, 0, 8, cg * 512, 512)
                addp((L, "gt", cg), "cf_w_pw1", j, 0, 8, 1024 + cg * 512, 512)
            for p in range(2):
                addp((L, "o", p), "cf_w_pw2", j, 0, 8, p * 512, 512)
        else:
            for cg in range(2):
                addp((L, "gc", cg), "sc_w_in", j, 0, 8, 1024 + cg * 512, 512)
                addp((L, "hh", cg), "sc_w_in", j, 0, 8, 2048 + cg * 512, 512)
                addp((L, "gb", cg), "sc_w_in", j, 0, 8, cg * 512, 512)
            for p in range(2):
                addp((L, "o", p), "sc_w_out", j, 0, 8, p * 512, 512)
        for p in range(8):
            addp((L, "w1", p), "mlp_w1", L, 0, 8, p * 512, 512)
        for p in range(8):
            addp((L, "w2", p), "mlp_w2", L, 0, 32, p * 128, 128)
    return pieces, index


def host_consts():
    lg = np.log1p(-np.exp2(-5.0 - np.arange(4, dtype=np.float32))).astype(np.float32).astype(np.float64)
    p = np.arange(128, dtype=np.float64)
    c = np.zeros((128, 176), np.float32)
    c[:, 0:128] = np.eye(128, dtype=np.float32)
    inv_freq = (np.float32(10000.0) ** (-np.arange(0, 256, 2, dtype=np.float32) / np.float32(256))).astype(np.float32)
    c[:, 128] = inv_freq
    for jc in range(4):
        for h in range(4):
            c[:, 129 + jc * 4 + h] = np.exp(-lg[h] * (jc * 128 + p + 1)) / 16.0
            c[:, 145 + jc * 4 + h] = np.exp(lg[h] * (511 - jc * 128 - p)) / 16.0
    c[:, 161] = -0.5
    qdec = np.exp(lg[:, None] * (np.arange(512, dtype=np.float64)[None, :] + 1.0)).astype(np.float32)
    qdec = np.ascontiguousarray(np.broadcast_to(qdec.reshape(1, 2048), (128, 2048))).astype(ml_dtypes.bfloat16)
    g512 = [float(np.exp(lg[h] * 512.0)) for h in range(4)]
    cb = np.zeros((128, 768), np.float32)
    cb[:, 0:128] = np.eye(128)
    cb[:, 128:256] = 1.0
    r = np.arange(512)[None, :]
    cb[:, 256:768] = (r >= np.arange(128)[:, None]).astype(np.float32)
    return c, qdec, g512, cb.astype(ml_dtypes.bfloat16)


CV_ID = 0
CV_INVF = 128
CV_KINV = 129
CV_KDEC = 145
CV_MHALF = 161
CV_P0 = 176
P_NORM = CV_P0
P_GN = P_NORM + 128
P_BPW1 = P_GN + 32
P_BDW = P_BPW1 + 16
P_LNG = P_BDW + 8
P_LNB = P_LNG + 8
P_BPW2 = P_LNB + 8
P_WDW = P_BPW2 + 8
P_SCW = P_WDW + 248
P_END = P_SCW + 24
CV_HB = P_END
CV_N = CV_HB + 8


def pack_params(inp):
    def fm(v):
        v = np.asarray(v, np.float32)
        lead = v.shape[:-1]
        n = v.shape[-1] // 128
        v = v.reshape(lead + (n, 128))
        v = np.moveaxis(v, -1, 0)
        return v.reshape(128, -1)

    cols = [
        fm(inp["norm_g"]),
        fm(inp["ret_gn_g"]),
        fm(inp["cf_b_pw1"][0]),
        fm(inp["cf_b_dw"][0]),
        fm(inp["cf_ln_g"][0]),
        fm(inp["cf_ln_b"][0]),
        fm(inp["cf_b_pw2"][0]),
        fm(inp["cf_w_dw"][0]),
        fm(inp["sc_w_conv"][0]),
    ]
    out = np.concatenate(cols, axis=1)
    assert out.shape[1] == P_END - CV_P0, out.shape
    return np.ascontiguousarray(out)


def build_program(ntiles=8, layers=(0, 1, 2, 3), g512=None):
    nc = bass.Bass("TRN2", target_bir_lowering=False)
    pieces, pindex = weight_pieces()
    NP = len(pieces)
    dram = {}
    dram["x"] = nc.dram_tensor("x", [S, D], F32, kind="ExternalInput").ap()
    dram["pos"] = nc.dram_tensor("pos", [1, S], I32, kind="ExternalInput").ap()
    dram["cf32"] = nc.dram_tensor("cf32", [128, CV_P0], F32, kind="ExternalInput").ap()
    dram["pvec"] = nc.dram_tensor("pvec", [128, P_END - CV_P0], F32, kind="ExternalInput").ap()
    dram["cb16"] = nc.dram_tensor("cb16", [128, 768], BF16, kind="ExternalInput").ap()
    dram["qdec"] = nc.dram_tensor("qdec", [128, 2048], BF16, kind="ExternalInput").ap()
    wshape = {"ret_w_in": [2, 1024, 6144], "ret_w_out": [2, 2048, 1024], "cf_w_pw1": [1, 1024, 2048],
              "cf_w_pw2": [1, 1024, 1024], "sc_w_in": [1, 1024, 3072], "sc_w_out": [1, 1024, 1024],
              "mlp_w1": [4, 1024, 4096], "mlp_w2": [4, 4096, 1024]}
    for k, shp in wshape.items():
        dram[k] = nc.dram_tensor(k, shp, F32, kind="ExternalInput").ap()
    wb = nc.dram_tensor("wb", [NP, 128, 4096], BF16, kind="Internal").ap()
    out = nc.dram_tensor("out", [S, D], F32, kind="ExternalOutput").ap()

    st = ExitStack()
    TR = Tracker()
    NU = 74
    AR = st.enter_context(nc.sbuf_tensor("AR", [128, NU * 512], BF16))
    WS = st.enter_context(nc.sbuf_tensor("WS", [128, NSLOT * 4096], BF16))
    XB = [st.enter_context(nc.sbuf_tensor(f"XB{i}", [128, NCH * T], F32)) for i in range(2)]
    NB = [st.enter_context(nc.sbuf_tensor(f"NB{i}", [128, NCH * T], BF16)) for i in range(2)]
    STT = [st.enter_context(nc.sbuf_tensor(f"ST{i}", [128, T], F32)) for i in range(2)]
    HR = st.enter_context(nc.sbuf_tensor("HR", [128, NCH * T], F32))
    SS = st.enter_context(nc.sbuf_tensor("SS", [128, 2 * 4 * 2 * 512], F32))
    CV = st.enter_context(nc.sbuf_tensor("CV", [128, CV_N], F32))
    CB = st.enter_context(nc.sbuf_tensor("CB", [128, 768], BF16))
    CFH = st.enter_context(nc.sbuf_tensor("CFH", [128, 8 * 32], BF16))
    SCH = st.enter_context(nc.sbuf_tensor("SCH", [128, 16], F32))
    QD = st.enter_context(nc.sbuf_tensor("QD", [128, 2048], BF16))
    DG = st.enter_context(nc.sbuf_tensor("DG", [128, 512], BF16))
    PS = st.enter_context(nc.psum_tensor("PS", [128, 8, 512], F32))

    ident_f = CV[:, CV_ID:CV_ID + 128]
    ident_b = CB[:, 0:128]
    ones_b = CB[:, 128:256]
    tri_b = CB[:, 256:768]

    def cvcol(c):
        return CV[:, c:c + 1]

    def abf(u, a=0, b=512):
        return AR[:, u * 512 + a:u * 512 + b]

    def af32(u, a=0, b=512):
        return AR[:, u * 512:(u + 2) * 512].bitcast(F32)[:, a:b]

    def ai32(u):
        return AR[:, u * 512:(u + 2) * 512].bitcast(I32)

    def ak(u, n=1):
        return [("A", u + i) for i in range(n)]

    bank_ctr = [0]

    reserved_banks = set()

    def nbank():
        while True:
            b = bank_ctr[0] % 8
            bank_ctr[0] += 1
            if b not in reserved_banks:
                return b

    def pk(b):
        return ("P", b)

    def mm_group(b, items, reads, a=0, bb=512):
        def fn(e, s):
            n = len(items)
            ins = None
            for i, (l, r) in enumerate(items):
                ins = e.matmul(PS[:, b, a:bb], lhsT=l, rhs=r, start=(i == 0), stop=(i == n - 1))
            return ins
        TR.add("pe", fn, reads=reads, writes=[pk(b)])

    def mm_multi(b, items, reads):
        def fn(e, s):
            n = len(items)
            ins = None
            for i, (o, l, r) in enumerate(items):
                ins = e.matmul(o, lhsT=l, rhs=r, start=(i == 0), stop=(i == n - 1))
            return ins
        TR.add("pe", fn, reads=reads, writes=[pk(b)])

    def act(out_, in_, func, reads, writes, bias=None, scale=None):
        kw = {}
        if bias is not None:
            kw["bias"] = bias
        if scale is not None:
            kw["scale"] = scale
        TR.add("act", lambda e, s: e.activation(out=out_, in_=in_, func=func, **kw), reads=reads, writes=writes)

    def vtt(eng, out_, in0, in1, op, reads, writes):
        TR.add(eng, lambda e, s: e.tensor_tensor(out=out_, in0=in0, in1=in1, op=op), reads=reads, writes=writes)

    def vts(eng, out_, in0, s1, s2, op0, op1, reads, writes):
        if s2 is None:
            TR.add(eng, lambda e, s: e.tensor_scalar(out=out_, in0=in0, scalar1=s1, scalar2=None, op0=op0),
                   reads=reads, writes=writes)
        else:
            TR.add(eng, lambda e, s: e.tensor_scalar(out=out_, in0=in0, scalar1=s1, scalar2=s2, op0=op0, op1=op1),
                   reads=reads, writes=writes)

    def vstt(out_, in0, scalar, in1, op0, op1, reads, writes):
        TR.add("dve", lambda e, s: e.scalar_tensor_tensor(out=out_, in0=in0, scalar=scalar, in1=in1, op0=op0, op1=op1),
               reads=reads, writes=writes)

    def vcopy(eng, out_, in_, reads, writes):
        TR.add(eng, lambda e, s: e.tensor_copy(out=out_, in_=in_), reads=reads, writes=writes)

    wctr = [0]

    def wload(key):
        pidx = pindex[key]
        slot = wctr[0] % NSLOT
        wctr[0] += 1
        ne = pieces[pidx][3] * pieces[pidx][5]
        TR.add("sp", lambda e, s: e.dma_start(out=WS[:, slot * 4096:slot * 4096 + ne], in_=wb[pidx][:, 0:ne]).then_inc(s, 16),
               reads=[("wb", pidx)], writes=[("W", slot)], dma=f"w{slot}")
        return slot

    def wap(slot, kc, ncols, c0, n=128):
        base = slot * 4096 + kc * ncols + c0
        return WS[:, base:base + n]

    def _cload(e, s):
        e.dma_start(out=CV[:, 0:CV_P0], in_=dram["cf32"][:, :]).then_inc(s, 16)
        e.dma_start(out=CV[:, CV_P0:P_END], in_=dram["pvec"][:, :]).then_inc(s, 16)
        e.dma_start(out=CB[:, :], in_=dram["cb16"][:, :]).then_inc(s, 16)
        e.dma_start(out=QD[:, :], in_=dram["qdec"][:, :]).then_inc(s, 16)
    TR.add("sp", _cload, writes=["C"], dma="c0", ndma=4)
    TR.add("pool", lambda e, s: e.memset(SS[:, :], 0.0), writes=[("S", r, h, dc) for r in range(2) for h in range(4) for dc in range(2)])
    TR.add("pool", lambda e, s: e.memset(CFH[:, :], 0.0), writes=[("CFH", c) for c in range(8)])
    TR.add("pool", lambda e, s: e.memset(SCH[:, :], 0.0), writes=[("SCH", c) for c in range(8)])
    vts("dve", CV[:, CV_HB:CV_HB + 8], CV[:, P_BPW1 + 8:P_BPW1 + 16], 0.5, None, ALU.mult, None, ["C"], ["C2"])

    cgrp = {"id": 0, "n": 0, "ops": [], "total": 0}

    def close_group():
        if cgrp["ops"]:
            last = cgrp["ops"][-1][1]
            for pi_, _ in cgrp["ops"]:
                TR.last_w[("wb", pi_)] = last
        cgrp["id"] += 1
        cgrp["n"] = 0
        cgrp["ops"] = []

    def cast_piece(pi):
        name, j, kc0, nkc, c0, ncols = pieces[pi]
        grp = cgrp["id"]
        src = dram[name][j, kc0 * 128:(kc0 + nkc) * 128, c0:c0 + ncols].rearrange("(k p) c -> p k c", p=128)
        dst = wb[pi][:, 0:nkc * ncols].rearrange("p (k c) -> p k c", c=ncols)
        rd = [("wb", cast_order[i]) for i in range(4)] if cgrp["total"] == 4 else []
        op = TR.add("pool", lambda e, s: e.dma_start(out=dst, in_=src).then_inc(s, 16), reads=rd, writes=[("wb", pi)], dma=f"cs{grp}")
        cgrp["ops"].append((pi, op))
        cgrp["n"] += 1
        cgrp["total"] += 1
        if cgrp["total"] <= 4 or cgrp["n"] >= 6:
            close_group()

    used_set = set()
    for k, pi in pindex.items():
        if k[0] in layers:
            used_set.add(pi)
    cast_order = [pi for pi in range(NP) if pi in used_set]
    cast_state = {"next": 0}

    def flush_casts(upto=None):
        n = len(cast_order) if upto is None else min(upto, len(cast_order))
        while cast_state["next"] < n:
            cast_piece(cast_order[cast_state["next"]])
            cast_state["next"] += 1
        close_group()

    store_ops = []

    def Xc(sl, c, a=0, b=T):
        return XB[sl][:, c * T + a:c * T + b]

    def Nc(sl, c, a=0, b=T):
        return NB[sl][:, c * T + a:c * T + b]

    def Hc(c, a=0, b=T):
        return HR[:, c * T + a:c * T + b]

    def STc(sl, i):
        return STT[sl][:, i * T:(i + 1) * T]

    def norm_g_col(L, n, c):
        return cvcol(P_NORM + (L * 4 + n) * 8 + c)

    def rms_stats(sl, src_ap, src_key):
        for c in range(NCH):
            act(Nc(sl, c), src_ap(c), AF.Square, [src_key(c)], [("N", sl, c)])
            if c % 2 == 1:
                yield None
        for _ in range(5):
            yield None
        b = nbank()
        mm_group(b, [(ones_b, Nc(sl, c)) for c in range(NCH)], ["C"] + [("N", sl, c) for c in range(NCH)])
        act(STc(sl, 0), PS[:, b, :], AF.Sqrt, [pk(b)], [("ST", sl, 0)], bias=RMS_EPS, scale=1.0 / D)
        for _ in range(3):
            yield None
        TR.add("dve", lambda e, s: e.reciprocal(out=STc(sl, 0), in_=STc(sl, 0)), reads=[("ST", sl, 0)], writes=[("ST", sl, 0)])
        for _ in range(3):
            yield None

    def pre_phase(sl, L, n):
        yield from rms_stats(sl, lambda c: Xc(sl, c), lambda c: ("X", sl, c))
        for c in range(NCH):
            vstt(Nc(sl, c), Xc(sl, c), norm_g_col(L, n, c), STc(sl, 0), ALU.mult, ALU.mult,
                 [("X", sl, c), ("ST", sl, 0), "C"], [("N", sl, c)])
            if c % 2 == 1:
                yield None

    def post_phase(sl, L, n):
        yield from rms_stats(sl, lambda c: Hc(c), lambda c: ("H", c))
        for c in range(NCH):
            vstt(Hc(c), Hc(c), norm_g_col(L, n, c), STc(sl, 0), ALU.mult, ALU.mult,
                 [("H", c), ("ST", sl, 0), "C"], [("H", c)])
            vtt("pool" if c % 2 else "dve", Xc(sl, c), Xc(sl, c), Hc(c), ALU.add, [("H", c), ("X", sl, c)], [("X", sl, c)])
            yield None

    U_STG = 0
    U_POS = 16

    def load_phase(sl, t):
        xk = [("X", sl, c) for c in range(NCH)]
        TR.add("pool", lambda e, s: e.dma_start(
            out=XB[sl][:, :].rearrange("p (j d) -> p j d", d=D),
            in_=dram["x"][t * T:(t + 1) * T, :].rearrange("(j p) d -> p j d", p=128)).then_inc(s, 16),
            writes=xk, dma="xl")
        for _ in range(14):
            yield None
        banks = [nbank() for _ in range(NCH)]
        for c in range(NCH):
            b = banks[c]

            def fn(e, s, b=b, c=c):
                ins = None
                for j in range(4):
                    ins = e.transpose(out=PS[:, b, j * 128:(j + 1) * 128], in_=XB[sl][:, j * D + c * 128:j * D + (c + 1) * 128], identity=ident_f)
                return ins
            TR.add("pe", fn, reads=xk + ["C"], writes=[pk(b)])
        for c in range(NCH):
            b = banks[c]
            if c % 2:
                act(Xc(sl, c), PS[:, b, :], AF.Copy, [pk(b)], [("X", sl, c)])
            else:
                vcopy("dve", Xc(sl, c), PS[:, b, :], [pk(b)], [("X", sl, c)])
        yield None

    U_CS = 70

    def cossin_phase(t):
        U_POS = 40
        TR.add("sp", lambda e, s: e.dma_start(out=ai32(U_POS), in_=dram["pos"][0:1, t * T:(t + 1) * T].partition_broadcast(128)).then_inc(s, 16),
               writes=ak(U_POS, 2), dma="pl")
        pf, ang, tq, kf, rr = (af32(U_POS + 2), af32(U_POS + 4), af32(U_POS + 6), af32(U_POS + 8), af32(U_POS + 10))
        ki = ai32(U_POS + 12)
        k_pf, k_ang, k_tq, k_kf, k_rr, k_ki = (ak(U_POS + 2, 2), ak(U_POS + 4, 2), ak(U_POS + 6, 2), ak(U_POS + 8, 2),
                                               ak(U_POS + 10, 2), ak(U_POS + 12, 2))
        vcopy("dve", pf, ai32(U_POS), ak(U_POS, 2), k_pf)
        vts("dve", ang, pf, cvcol(CV_INVF), None, ALU.mult, None, k_pf + ["C"], k_ang)
        for which in range(2):
            if which == 0:
                vts("dve", tq, ang, 1.0 / TWO_PI, None, ALU.mult, None, k_ang, k_tq)
            else:
                vts("dve", tq, ang, 1.0 / TWO_PI, 0.25, ALU.mult, ALU.add, k_ang, k_tq)
            vcopy("dve", ki, tq, k_tq, k_ki)
            vcopy("dve", kf, ki, k_ki, k_kf)
            vstt(rr, kf, -CW1, ang, ALU.mult, ALU.add, k_kf + k_ang, k_rr)
            vstt(rr, kf, -CW2, rr, ALU.mult, ALU.add, k_kf + k_rr, k_rr)
            if which == 1:
                vts("dve", rr, rr, math.pi / 2, None, ALU.add, None, k_rr, k_rr)
            vts("dve", rr, rr, -math.pi, math.pi, ALU.max, ALU.min, k_rr, k_rr)
            du = U_CS + 2 * (1 - which)
            act(af32(du), rr, AF.Sin, k_rr, ak(du, 2))
            yield None

    def store_phase(sl, t):
        xk = [("X", sl, c) for c in range(NCH)]
        banks = [nbank() for _ in range(NCH)]
        for j in range(4):
            for half in range(2):
                b = banks[j * 2 + half]

                def fn(e, s, b=b, j=j, half=half):
                    ins = None
                    for cc in range(4):
                        c = half * 4 + cc
                        ins = e.transpose(out=PS[:, b, cc * 128:(cc + 1) * 128], in_=Xc(sl, c, j * 128, (j + 1) * 128), identity=ident_f)
                    return ins
                TR.add("pe", fn, reads=xk + ["C"], writes=[pk(b)])
        for j in range(4):
            for half in range(2):
                b = banks[j * 2 + half]
                dst = XB[sl][:, j * D + half * 512:j * D + (half + 1) * 512]
                if half:
                    act(dst, PS[:, b, :], AF.Copy, [pk(b)], [("X", sl, j * 2 + half)])
                else:
                    vcopy("dve", dst, PS[:, b, :], [pk(b)], [("X", sl, j * 2 + half)])
        op = TR.add("pool", lambda e, s: e.dma_start(
            out=out[t * T:(t + 1) * T, :].rearrange("(j p) d -> p j d", p=128),
            in_=XB[sl][:, :].rearrange("p (j d) -> p j d", d=D)).then_inc(s, 16),
            reads=xk, dma="xs")
        store_ops.append(op)
        yield None

    U_H = 0
    U_R = 32

    def mlp_phase(sl, L):
        rctr = 0
        for p in range(8):
            slot = wload((L, "w1", p))
            for ocl in range(4):
                oc = p * 4 + ocl
                b = nbank()
                mm_group(b, [(wap(slot, kc, 512, ocl * 128), Nc(sl, kc)) for kc in range(NCH)],
                         [("W", slot)] + [("N", sl, kc) for kc in range(NCH)])
                ru = U_R + 2 * (rctr % 3)
                rctr += 1
                act(af32(ru), PS[:, b, :], AF.Relu, [pk(b)], ak(ru, 2))
                vtt("pool" if oc % 3 == 2 else "dve", abf(U_H + oc), af32(ru), af32(ru), ALU.mult, ak(ru, 2), ak(U_H + oc))
                yield None
        yield "sync"
        for dc in range(8):
            slot = wload((L, "w2", dc))
            b = nbank()
            mm_group(b, [(wap(slot, hc, 128, 0), abf(U_H + hc)) for hc in range(32)], [("W", slot)] + ak(U_H, 32))
            if dc % 2:
                act(Hc(dc), PS[:, b, :], AF.Copy, [pk(b)], [("H", dc)])
            else:
                vcopy("dve", Hc(dc), PS[:, b, :], [pk(b)], [("H", dc)])
            yield None

    U_SC_Y = 0
    U_SC_U = 8
    U_SC_T = 14
    U_SC_C = 18

    def sc_phase(sl, L):
        j = L // 3
        ctr = 0
        for cg in range(2):
            s_gc = wload((L, "gc", cg))
            s_hh = wload((L, "hh", cg))
            s_gb = wload((L, "gb", cg))
            for cl in range(4):
                c = cg * 4 + cl
                bq = {}
                for nm, slot in (("gc", s_gc), ("hh", s_hh), ("gb", s_gb)):
                    b = nbank()
                    bq[nm] = b
                    mm_group(b, [(wap(slot, kc, 512, cl * 128), Nc(sl, kc)) for kc in range(NCH)],
                             [("W", slot)] + [("N", sl, kc) for kc in range(NCH)])
                par = ctr % 2
                ctr += 1
                uu = U_SC_U + 3 * par
                tt = U_SC_T + 2 * par
                cc = U_SC_C + 2 * par
                Uf = AR[:, uu * 512:(uu + 3) * 512].bitcast(F32)
                ukeys = ak(uu, 3)
                act(af32(tt), PS[:, bq["hh"], :], AF.Copy, [pk(bq["hh"])], ak(tt, 2))
                vcopy("pool", Uf[:, 0:2], SCH[:, 2 * c:2 * c + 2], [("SCH", c)], ukeys)
                vtt("dve", Uf[:, 2:514], PS[:, bq["gc"], :], af32(tt), ALU.mult, [pk(bq["gc"])] + ak(tt, 2), ukeys)
                vcopy("pool", SCH[:, 2 * c:2 * c + 2], Uf[:, 512:514], ukeys, [("SCH", c)])
                w0, w1, w2 = (cvcol(P_SCW + k * 8 + c) for k in range(3))
                vts("dve", af32(cc), Uf[:, 0:512], w0, None, ALU.mult, None, ukeys + ["C"], ak(cc, 2))
                vstt(af32(cc), Uf[:, 1:513], w1, af32(cc), ALU.mult, ALU.add, ukeys + ak(cc, 2) + ["C"], ak(cc, 2))
                vstt(af32(cc), Uf[:, 2:514], w2, af32(cc), ALU.mult, ALU.add, ukeys + ak(cc, 2) + ["C"], ak(cc, 2))
                vtt("dve", abf(U_SC_Y + c), PS[:, bq["gb"], :], af32(cc), ALU.mult, [pk(bq["gb"])] + ak(cc, 2), ak(U_SC_Y + c))
                yield None
        yield "sync"
        for p in range(2):
            slot = wload((L, "o", p))
            for dcl in range(4):
                dc = p * 4 + dcl
                b = nbank()
                mm_group(b, [(wap(slot, kc, 512, dcl * 128), abf(U_SC_Y + kc)) for kc in range(NCH)],
                         [("W", slot)] + ak(U_SC_Y, 8))
                if dc % 2:
                    act(Hc(dc), PS[:, b, :], AF.Copy, [pk(b)], [("H", dc)])
                else:
                    vcopy("dve", Hc(dc), PS[:, b, :], [pk(b)], [("H", dc)])
                yield None

    U_CF_G = 0
    U_CF_HC = 16
    U_CF_HB = 32
    U_CF_HQ = 40
    U_CF_HS = 48
    U_CF_T = 56
    U_CF_D = 68

    def cf_phase(sl, L):
        ctr = 0

        def Gap(c, a, b):
            return AR[:, (U_CF_G + 2 * c) * 512 + a:(U_CF_G + 2 * c) * 512 + b]
        for cg in range(2):
            s_a = wload((L, "a", cg))
            s_g = wload((L, "gt", cg))
            for cl in range(4):
                c = cg * 4 + cl
                ba = nbank()
                mm_group(ba, [(wap(s_a, kc, 512, cl * 128), Nc(sl, kc)) for kc in range(NCH)],
                         [("W", s_a)] + [("N", sl, kc) for kc in range(NCH)])
                bg = nbank()
                mm_group(bg, [(wap(s_g, kc, 512, cl * 128), Nc(sl, kc)) for kc in range(NCH)],
                         [("W", s_g)] + [("N", sl, kc) for kc in range(NCH)])
                tu = U_CF_T + 2 * (ctr % 2)
                ctr += 1
                act(af32(tu), PS[:, bg, :], AF.Tanh, [pk(bg), "C2"], ak(tu, 2), bias=cvcol(CV_HB + c), scale=0.5)
                vts("pool", af32(tu), af32(tu), 0.5, 0.5, ALU.mult, ALU.add, ak(tu, 2), ak(tu, 2))
                gk = ak(U_CF_G + 2 * c, 2)
                vcopy("pool", Gap(c, 0, 30), CFH[:, c * 32:c * 32 + 30], [("CFH", c)], gk)
                vstt(Gap(c, 30, 542), PS[:, ba, :], cvcol(P_BPW1 + c), af32(tu), ALU.add, ALU.mult,
                     [pk(ba), "C"] + ak(tu, 2), gk)
                vcopy("pool", CFH[:, c * 32:c * 32 + 30], Gap(c, 512, 542), gk, [("CFH", c)])
                yield None
        dctr = 0
        for c in range(NCH):
            b = nbank()
            gk = ak(U_CF_G + 2 * c, 2)
            for k in range(31):
                ds = dctr % 4
                dctr += 1
                dap = DG[:, ds * 128:(ds + 1) * 128]
                dkey = ("DG", ds)
                vts("pool", dap, ident_b, cvcol(P_WDW + k * 8 + c), 0.0, ALU.mult, ALU.add, ["C"], [dkey])
                TR.add("pe", lambda e, s, b=b, dap=dap, c=c, k=k: e.matmul(PS[:, b, :], lhsT=dap, rhs=Gap(c, k, k + 512),
                                                                            start=(k == 0), stop=(k == 30)),
                       reads=[dkey] + gk, writes=[pk(b)])
                if k % 4 == 3:
                    yield None
            act(af32(U_CF_HC + 2 * c), PS[:, b, :], AF.Identity, [pk(b), "C"], ak(U_CF_HC + 2 * c, 2), bias=cvcol(P_BDW + c))
            act(abf(U_CF_HB + c), PS[:, b, :], AF.Identity, [pk(b), "C"], ak(U_CF_HB + c), bias=cvcol(P_BDW + c))
            act(abf(U_CF_HQ + c), PS[:, b, :], AF.Square, [pk(b), "C"], ak(U_CF_HQ + c), bias=cvcol(P_BDW + c))
            yield None
        b1 = nbank()
        mm_group(b1, [(ones_b, abf(U_CF_HB + c)) for c in range(NCH)], ["C"] + ak(U_CF_HB, 8))
        b2 = nbank()
        mm_group(b2, [(ones_b, abf(U_CF_HQ + c)) for c in range(NCH)], ["C"] + ak(U_CF_HQ, 8))
        mean, msq, rstd, bm = (U_CF_T + 4, U_CF_T + 6, U_CF_T + 8, U_CF_T + 10)
        act(af32(mean), PS[:, b1, :], AF.Copy, [pk(b1)], ak(mean, 2), scale=1.0 / D)
        vtt("dve", af32(msq), af32(mean), af32(mean), ALU.mult, ak(mean, 2), ak(msq, 2))
        vstt(af32(msq), PS[:, b2, :], 1.0 / D, af32(msq), ALU.mult, ALU.subtract, [pk(b2)] + ak(msq, 2), ak(msq, 2))
        act(af32(rstd), af32(msq), AF.Sqrt, ak(msq, 2), ak(rstd, 2), bias=LN_EPS)
        TR.add("dve", lambda e, s: e.reciprocal(out=af32(rstd), in_=af32(rstd)), reads=ak(rstd, 2), writes=ak(rstd, 2))
        vstt(af32(bm), af32(mean), -1.0, af32(rstd), ALU.mult, ALU.mult, ak(mean, 2) + ak(rstd, 2), ak(bm, 2))
        yield None
        for c in range(NCH):
            hk = ak(U_CF_HC + 2 * c, 2)
            eng = "pool" if c % 2 else "dve"
            vtt(eng, af32(U_CF_HC + 2 * c), af32(U_CF_HC + 2 * c), af32(rstd), ALU.mult, hk + ak(rstd, 2), hk)
            vtt(eng, af32(U_CF_HC + 2 * c), af32(U_CF_HC + 2 * c), af32(bm), ALU.add, hk + ak(bm, 2), hk)
            act(abf(U_CF_HS + c), af32(U_CF_HC + 2 * c), AF.Silu, hk + ["C"], ak(U_CF_HS + c),
                bias=cvcol(P_LNB + c), scale=cvcol(P_LNG + c))
            yield None
        yield "sync"
        for p in range(2):
            slot = wload((L, "o", p))
            for dcl in range(4):
                dc = p * 4 + dcl
                b = nbank()
                mm_group(b, [(wap(slot, kc, 512, dcl * 128), abf(U_CF_HS + kc)) for kc in range(NCH)],
                         [("W", slot)] + ak(U_CF_HS, 8))
                act(Hc(dc), PS[:, b, :], AF.Identity, [pk(b), "C"], [("H", dc)], bias=cvcol(P_BPW2 + dc))
                yield None

    U_RT_SET = 0
    U_RT_KT = 24
    U_RT_SC = 26
    U_RT_Y = 30
    U_RT_YQ = 34
    U_RT_SB = 38
    U_RT_YG = 40
    U_RT_F = 56

    def ret_phase(sl, L):
        r = L // 3
        cos_t = af32(U_CS)
        sin_t = af32(U_CS + 2)
        ck = ak(U_CS, 4)
        fa, fb, fcq, fsq = U_RT_F, U_RT_F + 2, U_RT_F + 4, U_RT_F + 6
        mean, msq, rstd = U_RT_F + 8, U_RT_F + 10, U_RT_F + 12
        bm = msq
        xnk = [("N", sl, kc) for kc in range(NCH)]

        def sbase(h):
            return U_RT_SET + 12 * (h % 2)

        def Sap(h, dc):
            o = ((r * 4 + h) * 2 + dc) * 512
            return SS[:, o:o + 512]

        def rotary(bt1, bt2, ta, tb, out1, out2, ok1, ok2):
            vtt("dve", af32(ta), PS[:, bt1, :], cos_t, ALU.mult, [pk(bt1)] + ck, ak(ta, 2))
            vtt("dve", af32(tb), PS[:, bt2, :], sin_t, ALU.mult, [pk(bt2)] + ck, ak(tb, 2))
            vtt("pool", out1, af32(ta), af32(tb), ALU.subtract, ak(ta, 2) + ak(tb, 2), ok1)
            vtt("dve", af32(ta), PS[:, bt2, :], cos_t, ALU.mult, [pk(bt2)] + ck, ak(ta, 2))
            vtt("dve", af32(tb), PS[:, bt1, :], sin_t, ALU.mult, [pk(bt1)] + ck, ak(tb, 2))
            vtt("pool", out2, af32(ta), af32(tb), ALU.add, ak(ta, 2) + ak(tb, 2), ok2)

        slots = {}

        def proj(h):
            sb = sbase(h)
            qd, kT, v, sg = sb, sb + 2, sb + 4, sb + 8
            slots["q"] = wload((L, "q", h))
            slots["k"] = wload((L, "k", h))
            for nm, dstu, ta, tb in (("q", qd, fa, fb), ("k", kT, fcq, fsq)):
                slot = slots[nm]
                b1 = nbank()
                mm_group(b1, [(wap(slot, kc, 256, 0), Nc(sl, kc)) for kc in range(NCH)], [("W", slot)] + xnk)
                b2 = nbank()
                mm_group(b2, [(wap(slot, kc, 256, 128), Nc(sl, kc)) for kc in range(NCH)], [("W", slot)] + xnk)
                rotary(b1, b2, ta, tb, abf(dstu), abf(dstu + 1), ak(dstu), ak(dstu + 1))
                yield None
            slot = wload((L, "v", h))
            for jc in range(4):
                b = nbank()
                mm_group(b, [(Nc(sl, kc, jc * 128, (jc + 1) * 128), wap(slot, kc, 512, 0, 512)) for kc in range(NCH)],
                         [("W", slot)] + xnk)
                act(abf(v + jc), PS[:, b, :], AF.Copy, [pk(b)], ak(v + jc))
                yield None
            slot = wload((L, "g", h))
            for cl in range(4):
                b = nbank()
                mm_group(b, [(wap(slot, kc, 512, cl * 128), Nc(sl, kc)) for kc in range(NCH)], [("W", slot)] + xnk)
                act(abf(sg + cl), PS[:, b, :], AF.Silu, [pk(b)], ak(sg + cl))
                yield None

        def attn(h):
            sb = sbase(h)
            qd, kT, v, sg = sb, sb + 2, sb + 4, sb + 8
            for jc in range(4):
                n = 512 - jc * 128
                b = nbank()
                mm_group(b, [(abf(kT + dc, jc * 128, (jc + 1) * 128), abf(qd + dc, jc * 128, 512)) for dc in range(2)],
                         ak(kT, 2) + ak(qd, 2), 0, n)
                vstt(abf(U_RT_SC + jc, 0, n), PS[:, b, 0:n], cvcol(CV_KINV + jc * 4 + h), tri_b[:, 0:n], ALU.mult, ALU.mult,
                     [pk(b), "C"], ak(U_RT_SC + jc))
            yield None
            b = nbank()
            pb = PS[:, b, :].bitcast(BF16)

            def fn(e, s):
                ins = None
                for jc in range(4):
                    for dc in range(2):
                        ins = e.transpose(out=pb[:, jc * 256 + dc * 128:jc * 256 + (dc + 1) * 128],
                                          in_=abf(kT + dc, jc * 128, (jc + 1) * 128), identity=ident_b)
                return ins
            TR.add("pe", fn, reads=ak(kT, 2) + ["C"], writes=[pk(b)])
            for jc in range(4):
                act(AR[:, U_RT_KT * 512 + jc * 256:U_RT_KT * 512 + (jc + 1) * 256], pb[:, jc * 256:(jc + 1) * 256], AF.Copy,
                    [pk(b), "C"], ak(U_RT_KT, 2), scale=cvcol(CV_KDEC + jc * 4 + h))
            yield None
            for dc in range(2):
                act(abf(U_RT_SB + dc), Sap(h, dc), AF.Copy, [("S", r, h, dc)], ak(U_RT_SB + dc))
            for dvc in range(4):
                b = nbank()
                items = []
                for dc in range(2):
                    items.append((PS[:, b, :], abf(U_RT_SB + dc, dvc * 128, (dvc + 1) * 128), abf(qd + dc)))
                for jc in range(4):
                    items.append((PS[:, b, jc * 128:512], abf(v + jc, dvc * 128, (dvc + 1) * 128), abf(U_RT_SC + jc, 0, 512 - jc * 128)))
                mm_multi(b, items, ak(U_RT_SB, 2) + ak(qd, 2) + ak(v, 4) + ak(U_RT_SC, 4))
                vtt("dve", abf(U_RT_Y + dvc), PS[:, b, :], QD[:, h * 512:(h + 1) * 512], ALU.mult, [pk(b), "C"], ak(U_RT_Y + dvc))
                act(abf(U_RT_YQ + dvc), abf(U_RT_Y + dvc), AF.Square, ak(U_RT_Y + dvc), ak(U_RT_YQ + dvc))
                yield None
            for dc in range(2):
                b = nbank()
                mm_group(b, [(AR[:, U_RT_KT * 512 + jc * 256 + dc * 128:U_RT_KT * 512 + jc * 256 + (dc + 1) * 128], abf(v + jc))
                             for jc in range(4)], ak(U_RT_KT, 2) + ak(v, 4))
                vstt(Sap(h, dc), Sap(h, dc), g512[h], PS[:, b, :], ALU.mult, ALU.add, [pk(b), ("S", r, h, dc)], [("S", r, h, dc)])
            yield None
            b1 = nbank()
            mm_group(b1, [(ones_b, abf(U_RT_Y + c)) for c in range(4)], ["C"] + ak(U_RT_Y, 4))
            b2 = nbank()
            mm_group(b2, [(ones_b, abf(U_RT_YQ + c)) for c in range(4)], ["C"] + ak(U_RT_YQ, 4))
            act(af32(mean), PS[:, b1, :], AF.Copy, [pk(b1)], ak(mean, 2), scale=1.0 / 512)
            vtt("dve", af32(msq), af32(mean), af32(mean), ALU.mult, ak(mean, 2), ak(msq, 2))
            vstt(af32(msq), PS[:, b2, :], 1.0 / 512, af32(msq), ALU.mult, ALU.subtract, [pk(b2)] + ak(msq, 2), ak(msq, 2))
            act(af32(rstd), af32(msq), AF.Sqrt, ak(msq, 2), ak(rstd, 2), bias=LN_EPS)
            TR.add("dve", lambda e, s: e.reciprocal(out=af32(rstd), in_=af32(rstd)), reads=ak(rstd, 2), writes=ak(rstd, 2))
            vstt(af32(bm), af32(mean), -1.0, af32(rstd), ALU.mult, ALU.mult, ak(mean, 2) + ak(rstd, 2), ak(bm, 2))
            yield None
            for c in range(4):
                tmp = fa if c % 2 == 0 else fb
                vtt("pool", af32(tmp), abf(U_RT_Y + c), af32(rstd), ALU.mult, ak(U_RT_Y + c) + ak(rstd, 2), ak(tmp, 2))
                vtt("dve", af32(tmp), af32(tmp), af32(bm), ALU.add, ak(tmp, 2) + ak(bm, 2), ak(tmp, 2))
                vstt(abf(U_RT_YG + h * 4 + c), af32(tmp), cvcol(P_GN + r * 16 + h * 4 + c), abf(sg + c), ALU.mult, ALU.mult,
                     ak(tmp, 2) + ak(sg + c) + ["C"], ak(U_RT_YG + h * 4 + c))
            yield None

        yield from cossin_phase(tile_of_slot[sl])
        yield from proj(0)
        carry = None
        for h in range(4):
            ga = attn(h)
            gp = proj(h + 1) if h + 1 < 4 else iter(())
            alive = {"A": True, "P": h + 1 < 4, "C": carry is not None}
            gens_ = {"A": ga, "P": gp, "C": carry}
            na = 0
            for ch in "PPAPCPAPPAAPAAPAPAP":
                if not alive[ch]:
                    continue
                if ch == "A":
                    if na >= 8:
                        continue
                    na += 1
                try:
                    next(gens_[ch])
                except StopIteration:
                    alive[ch] = False
                    continue
                yield None
            for ch in "CP":
                while alive[ch]:
                    try:
                        next(gens_[ch])
                    except StopIteration:
                        alive[ch] = False
                        break
                    yield None
            while na < 8:
                next(ga)
                na += 1
                yield None
            carry = ga
        early = {}
        oslots = {}
        for p in range(2):
            oslots[p] = wload((L, "o", p))
            for dcl in range(2):
                dc = p * 2 + dcl
                b = nbank()
                reserved_banks.add(b)
                early[dc] = b

                def fnp(e, s, b=b, slot=oslots[p], dcl=dcl):
                    ins = None
                    for kc in range(12):
                        ins = e.matmul(PS[:, b, :], lhsT=wap(slot, kc, 256, dcl * 128), rhs=abf(U_RT_YG + kc), start=(kc == 0), stop=False)
                    return ins
                TR.add("pe", fnp, reads=[("W", oslots[p])] + ak(U_RT_YG, 12), writes=[pk(b)])
            yield None
        for _ in carry:
            yield None
        yield "sync"
        for p in range(4):
            slot = oslots[p] if p < 2 else wload((L, "o", p))
            for dcl in range(2):
                dc = p * 2 + dcl
                if dc in early:
                    b = early[dc]

                    def fnf(e, s, b=b, slot=slot, dcl=dcl):
                        ins = None
                        for kc in range(12, 16):
                            ins = e.matmul(PS[:, b, :], lhsT=wap(slot, kc, 256, dcl * 128), rhs=abf(U_RT_YG + kc), start=False, stop=(kc == 15))
                        return ins
                    TR.add("pe", fnf, reads=[("W", slot)] + ak(U_RT_YG + 12, 4), writes=[pk(b)])
                    reserved_banks.discard(b)
                else:
                    b = nbank()
                    mm_group(b, [(wap(slot, kc, 256, dcl * 128), abf(U_RT_YG + kc)) for kc in range(16)],
                             [("W", slot)] + ak(U_RT_YG, 16))
                if dc % 2:
                    act(Hc(dc), PS[:, b, :], AF.Copy, [pk(b)], [("H", dc)])
                else:
                    vcopy("dve", Hc(dc), PS[:, b, :], [pk(b)], [("H", dc)])
                yield None

    def mixer_phase(sl, L):
        kind = L % 3
        if kind == 0:
            yield from ret_phase(sl, L)
        elif kind == 1:
            yield from cf_phase(sl, L)
        else:
            yield from sc_phase(sl, L)

    tile_of_slot = [0, 0]

    def thread(sl, t):
        tile_of_slot[sl] = t
        yield from load_phase(sl, t)
        for L in layers:
            yield from pre_phase(sl, L, 0)
            yield ("acq", ("mm", L, 0))
            if t == 0:
                flush_casts(upto=phase_end[(L, 1)])
            yield from mixer_phase(sl, L)
            yield ("rel", ("mm", L, 0))
            yield from post_phase(sl, L, 1)
            yield from pre_phase(sl, L, 2)
            yield ("acq", ("mm", L, 1))
            if t == 0:
                nxt = phase_list.index((L, 1)) + 1
                if nxt < len(phase_list):
                    flush_casts(upto=phase_end[phase_list[nxt]])
            yield from mlp_phase(sl, L)
            yield ("rel", ("mm", L, 1))
            yield from post_phase(sl, L, 3)
        yield from store_phase(sl, t)

    phase_end = {}
    cnt = 0
    for L_ in layers:
        nmix = sum(1 for k in pindex if k[0] == L_ and k[1] not in ("w1", "w2"))
        cnt += nmix
        phase_end[(L_, 0)] = cnt
        cnt += 16
        phase_end[(L_, 1)] = cnt
    phase_list = [(L_, m_) for L_ in layers for m_ in (0, 1)]
    flush_casts(upto=4)
    first_ops = [TR.last_w[("wb", cast_order[i])] for i in range(min(4, len(cast_order)))]
    flush_casts(upto=phase_end[phase_list[0]])

    done = {}
    gens = [None, None]
    tiles_of = [None, None]
    pending = [None, None]
    next_tile = 0
    owner = None

    def advance(sl):
        nonlocal owner
        g = gens[sl]
        if g is None:
            return False
        t = tiles_of[sl]
        if pending[sl] is not None:
            pid = pending[sl]
            if owner is not None or (t > 0 and not done.get((t - 1, pid))):
                return False
            owner = sl
            pending[sl] = None
        try:
            v = next(g)
        except StopIteration:
            gens[sl] = None
            return False
        if isinstance(v, tuple):
            if v[0] == "acq":
                pending[sl] = v[1]
            else:
                done[(t, v[1])] = True
                owner = None
        elif v == "sync":
            o = 1 - sl
            while gens[o] is not None and pending[o] is None:
                if not advance(o):
                    break
        return True

    while True:
        for sl in range(2):
            if gens[sl] is None and next_tile < ntiles:
                gens[sl] = thread(sl, next_tile)
                tiles_of[sl] = next_tile
                pending[sl] = None
                next_tile += 1
        if gens[0] is None and gens[1] is None:
            break
        order = [owner, 1 - owner] if owner is not None else [0, 1]
        if owner is None and gens[0] is not None and gens[1] is not None and tiles_of[1] < tiles_of[0]:
            order = [1, 0]
        prog = False
        for oi, sl in enumerate(order):
            if advance(sl):
                prog = True
            if oi == 1 and owner is not None and owner != sl:
                for _ in range(1):
                    if advance(sl):
                        prog = True
        if not prog and (gens[0] is not None or gens[1] is not None):
            raise RuntimeError(f"scheduler deadlock owner={owner} pending={pending} tiles={tiles_of} gens={[g is not None for g in gens]} done={sorted(done, key=str)[-6:]}")

    flush_casts()
    nsig = TR.emit(nc, st, final_waits=store_ops)
    st.close()
    return nc, nsig, len(TR.ops)


_CACHE = {}


def kernel(x, positions, norm_g, ret_w_in, ret_gn_g, ret_w_out, cf_w_pw1, cf_b_pw1, cf_w_dw, cf_b_dw,
           cf_ln_g, cf_ln_b, cf_w_pw2, cf_b_pw2, sc_w_in, sc_w_conv, sc_w_out, mlp_w1, mlp_w2):
    inp = dict(norm_g=norm_g, ret_gn_g=ret_gn_g, cf_b_pw1=cf_b_pw1, cf_b_dw=cf_b_dw, cf_ln_g=cf_ln_g,
               cf_ln_b=cf_ln_b, cf_b_pw2=cf_b_pw2, cf_w_dw=cf_w_dw, sc_w_conv=sc_w_conv)
    inp = {k: np.asarray(v) for k, v in inp.items()}
    cf32, qdec, g512, cb16 = host_consts()
    pvec = pack_params(inp)
    if "nc" not in _CACHE:
        _CACHE["nc"] = build_program(ntiles=S // T, layers=(0, 1, 2, 3), g512=g512)[0]
    nc = _CACHE["nc"]
    x = np.asarray(x, np.float32)
    positions = np.asarray(positions, np.int32)
    B = x.shape[0]
    shared = {
        "cf32": cf32, "pvec": pvec, "cb16": cb16, "qdec": qdec,
        "ret_w_in": np.ascontiguousarray(ret_w_in, np.float32), "ret_w_out": np.ascontiguousarray(ret_w_out, np.float32),
        "cf_w_pw1": np.ascontiguousarray(cf_w_pw1, np.float32), "cf_w_pw2": np.ascontiguousarray(cf_w_pw2, np.float32),
        "sc_w_in": np.ascontiguousarray(sc_w_in, np.float32), "sc_w_out": np.ascontiguousarray(sc_w_out, np.float32),
        "mlp_w1": np.ascontiguousarray(mlp_w1, np.float32), "mlp_w2": np.ascontiguousarray(mlp_w2, np.float32),
    }
    in_maps = []
    for b in range(B):
        m = dict(shared)
        m["x"] = np.ascontiguousarray(x[b])
        m["pos"] = np.ascontiguousarray(positions[b][None, :])
        in_maps.append(m)
    res = run_bass_kernel_spmd(nc, in_maps, core_ids=list(range(B)))
    return np.stack([np.asarray(r["out"], np.float32) for r in res.results], axis=0)
```

```python
import math
from contextlib import ExitStack

import ml_dtypes
import numpy as np

import concourse.bass as bass
import concourse.mybir as mybir
from concourse.bass_utils import run_bass_kernel_spmd

F32 = mybir.dt.float32
BF16 = mybir.dt.bfloat16
I32 = mybir.dt.int32
AF = mybir.ActivationFunctionType
ALU = mybir.AluOpType

D = 1024
S = 4096
T = 512
NCH = 8
DEPTH = 4
NSLOT = 3
RMS_EPS = 1e-6
LN_EPS = 1e-5
TWO_PI = 2.0 * math.pi
CW1 = 6.28125
CW2 = TWO_PI - CW1

ENGS = ("pe", "act", "dve", "pool", "sp")
SEM_LIMIT = 30000


class Op:
    __slots__ = ("eng", "fn", "waits", "signal", "cseq", "comp", "vc", "ndma", "idx", "sigcount")


class Tracker:
    def __init__(self):
        self.ops = []
        self.eng_ops = {e: [] for e in ENGS}
        self.eng_clock = {e: {} for e in ENGS}
        self.last_w = {}
        self.readers = {}
        self.dma_count = {}

    def add(self, eng, fn, reads=(), writes=(), dma=None, ndma=1):
        op = Op()
        op.eng = eng
        op.fn = fn
        op.signal = False
        op.ndma = ndma
        op.idx = len(self.ops)
        elist = self.eng_ops[eng]
        if dma is None:
            op.comp = eng
            op.cseq = len(elist) + 1
        else:
            op.comp = ("dma", dma)
            self.dma_count[dma] = self.dma_count.get(dma, 0) + ndma
            op.cseq = self.dma_count[dma]
        deps = {}
        for k in reads:
            w = self.last_w.get(k)
            if w is not None:
                deps[w.idx] = (w, True)
        for k in writes:
            w = self.last_w.get(k)
            if w is not None and w.idx not in deps:
                deps[w.idx] = (w, False)
            for r in self.readers.get(k, ()):
                if r.idx not in deps:
                    deps[r.idx] = (r, False)
        clock = dict(self.eng_clock[eng])
        waits = []
        ncur = len(elist) + 1
        for di in sorted(deps):
            d, raw = deps[di]
            src, seq = d.comp, d.cseq
            if clock.get(src, 0) >= seq:
                continue
            if src == eng:
                if eng == "pe" or not raw:
                    continue
            waits.append(d)
            d.signal = True
            for s2, q2 in d.vc.items():
                if clock.get(s2, 0) < q2:
                    clock[s2] = q2
        op.waits = waits
        self.eng_clock[eng] = clock
        vc = dict(clock)
        vc[op.comp] = op.cseq
        op.vc = vc
        for k in reads:
            self.readers.setdefault(k, []).append(op)
        for k in writes:
            self.last_w[k] = op
            self.readers[k] = []
        self.ops.append(op)
        elist.append(op)
        return op

    def emit(self, nc, stack, final_waits=()):
        nsig = {}
        for e in ENGS:
            c = 0
            for op in self.eng_ops[e]:
                if op.comp == e and op.signal:
                    c += 1
                    op.sigcount = c
            nsig[e] = c
        sems = {}
        for e in ENGS:
            nep = (nsig[e] + SEM_LIMIT - 1) // SEM_LIMIT
            sems[e] = [stack.enter_context(nc.semaphore(f"s_{e}_{i}")) for i in range(max(nep, 1))]
        dsems = {}
        for name in self.dma_count:
            dsems[name] = stack.enter_context(nc.semaphore(f"d_{name}"))

        def semval(d):
            if isinstance(d.comp, tuple):
                return dsems[d.comp[1]], 16 * d.cseq
            c = d.sigcount
            return sems[d.comp][(c - 1) // SEM_LIMIT], (c - 1) % SEM_LIMIT + 1

        block = stack.enter_context(nc.Block())
        tr = self

        def run(engname):
            def body(eng):
                for op in tr.eng_ops[engname]:
                    for d in op.waits:
                        s, v = semval(d)
                        eng.wait_ge(s, v)
                    if isinstance(op.comp, tuple):
                        op.fn(eng, dsems[op.comp[1]])
                    else:
                        ins = op.fn(eng, None)
                        if op.signal:
                            s, v = semval(op)
                            ins.then_inc(s, 1)
                if engname == "pool":
                    for d in final_waits:
                        s, v = semval(d)
                        eng.wait_ge(s, v)
            return body

        block.tensor(run("pe"))
        block.scalar(run("act"))
        block.vector(run("dve"))
        block.gpsimd(run("pool"))
        block.sync(run("sp"))
        return nsig


def layer_kind(L):
    return L % 3, L // 3


def weight_pieces():
    pieces = []
    index = {}

    def addp(key, name, j, kc0, nkc, c0, ncols):
        index[key] = len(pieces)
        pieces.append((name, j, kc0, nkc, c0, ncols))

    for L in range(DEPTH):
        kind, j = layer_kind(L)
        if kind == 0:
            for h in range(4):
                addp((L, "q", h), "ret_w_in", j, 0, 8, h * 256, 256)
                addp((L, "k", h), "ret_w_in", j, 0, 8, 1024 + h * 256, 256)
                addp((L, "v", h), "ret_w_in", j, 0, 8, 2048 + h * 512, 512)
                addp((L, "g", h), "ret_w_in", j, 0, 8, 4096 + h * 512, 512)
            for p in range(4):
                addp((L, "o", p), "ret_w_out", j, 0, 16, p * 256, 256)
        elif kind == 1:
            for cg in range(2):
                addp((L, "a", cg), "cf_w_pw1", j, 0, 8, cg * 512, 512)
                addp((L, "gt", cg), "cf_w_pw1", j, 0, 8, 1024 + cg * 512, 512)
            for p in range(2):
                addp((L, "o", p), "cf_w_pw2", j, 0, 8, p * 512, 512)
        else:
            for cg in range(2):
                addp((L, "gc", cg), "sc_w_in", j, 0, 8, 1024 + cg * 512, 512)
                addp((L, "hh", cg), "sc_w_in", j, 0, 8, 2048 + cg * 512, 512)
                addp((L, "gb", cg), "sc_w_in", j, 0, 8, cg * 512, 512)
            for p in range(2):
                addp((L, "o", p), "sc_w_out", j, 0, 8, p * 512, 512)
        for p in range(8):
            addp((L, "w1", p), "mlp_w1", L, 0, 8, p * 512, 512)
        for p in range(8):
            addp((L, "w2", p), "mlp_w2", L, 0, 32, p * 128, 128)
    return pieces, index


def host_consts():
    lg = np.log1p(-np.exp2(-5.0 - np.arange(4, dtype=np.float32))).astype(np.float32).astype(np.float64)
    p = np.arange(128, dtype=np.float64)
    c = np.zeros((128, 176), np.float32)
    c[:, 0:128] = np.eye(128, dtype=np.float32)
    inv_freq = (np.float32(10000.0) ** (-np.arange(0, 256, 2, dtype=np.float32) / np.float32(256))).astype(np.float32)
    c[:, 128] = inv_freq
    for jc in range(4):
        for h in range(4):
            c[:, 129 + jc * 4 + h] = np.exp(-lg[h] * (jc * 128 + p + 1)) / 16.0
            c[:, 145 + jc * 4 + h] = np.exp(lg[h] * (511 - jc * 128 - p)) / 16.0
    c[:, 161] = -0.5
    qdec = np.exp(lg[:, None] * (np.arange(512, dtype=np.float64)[None, :] + 1.0)).astype(np.float32)
    qdec = np.ascontiguousarray(np.broadcast_to(qdec.reshape(1, 2048), (128, 2048))).astype(ml_dtypes.bfloat16)
    g512 = [float(np.exp(lg[h] * 512.0)) for h in range(4)]
    cb = np.zeros((128, 768), np.float32)
    cb[:, 0:128] = np.eye(128)
    cb[:, 128:256] = 1.0
    r = np.arange(512)[None, :]
    cb[:, 256:768] = (r >= np.arange(128)[:, None]).astype(np.float32)
    return c, qdec, g512, cb.astype(ml_dtypes.bfloat16)


CV_ID = 0
CV_INVF = 128
CV_KINV = 129
CV_KDEC = 145
CV_MHALF = 161
CV_P0 = 176
P_NORM = CV_P0
P_GN = P_NORM + 128
P_BPW1 = P_GN + 32
P_BDW = P_BPW1 + 16
P_LNG = P_BDW + 8
P_LNB = P_LNG + 8
P_BPW2 = P_LNB + 8
P_WDW = P_BPW2 + 8
P_SCW = P_WDW + 248
P_END = P_SCW + 24
CV_HB = P_END
CV_N = CV_HB + 8


def pack_params(inp):
    def fm(v):
        v = np.asarray(v, np.float32)
        lead = v.shape[:-1]
        n = v.shape[-1] // 128
        v = v.reshape(lead + (n, 128))
        v = np.moveaxis(v, -1, 0)
        return v.reshape(128, -1)

    cols = [
        fm(inp["norm_g"]),
        fm(inp["ret_gn_g"]),
        fm(inp["cf_b_pw1"][0]),
        fm(inp["cf_b_dw"][0]),
        fm(inp["cf_ln_g"][0]),
        fm(inp["cf_ln_b"][0]),
        fm(inp["cf_b_pw2"][0]),
        fm(inp["cf_w_dw"][0]),
        fm(inp["sc_w_conv"][0]),
    ]
    out = np.concatenate(cols, axis=1)
    assert out.shape[1] == P_END - CV_P0, out.shape
    return np.ascontiguousarray(out)


def build_program(ntiles=8, layers=(0, 1, 2, 3), g512=None):
    nc = bass.Bass("TRN2", target_bir_lowering=False)
    pieces, pindex = weight_pieces()
    NP = len(pieces)
    dram = {}
    dram["x"] = nc.dram_tensor("x", [S, D], F32, kind="ExternalInput").ap()
    dram["pos"] = nc.dram_tensor("pos", [1, S], I32, kind="ExternalInput").ap()
    dram["cf32"] = nc.dram_tensor("cf32", [128, CV_P0], F32, kind="ExternalInput").ap()
    dram["pvec"] = nc.dram_tensor("pvec", [128, P_END - CV_P0], F32, kind="ExternalInput").ap()
    dram["cb16"] = nc.dram_tensor("cb16", [128, 768], BF16, kind="ExternalInput").ap()
    dram["qdec"] = nc.dram_tensor("qdec", [128, 2048], BF16, kind="ExternalInput").ap()
    wshape = {"ret_w_in": [2, 1024, 6144], "ret_w_out": [2, 2048, 1024], "cf_w_pw1": [1, 1024, 2048],
              "cf_w_pw2": [1, 1024, 1024], "sc_w_in": [1, 1024, 3072], "sc_w_out": [1, 1024, 1024],
              "mlp_w1": [4, 1024, 4096], "mlp_w2": [4, 4096, 1024]}
    for k, shp in wshape.items():
        dram[k] = nc.dram_tensor(k, shp, F32, kind="ExternalInput").ap()
    wb = nc.dram_tensor("wb", [NP, 128, 4096], BF16, kind="Internal").ap()
    out = nc.dram_tensor("out", [S, D], F32, kind="ExternalOutput").ap()

    st = ExitStack()
    TR = Tracker()
    NU = 74
    AR = st.enter_context(nc.sbuf_tensor("AR", [128, NU * 512], BF16))
    WS = st.enter_context(nc.sbuf_tensor("WS", [128, NSLOT * 4096], BF16))
    XB = [st.enter_context(nc.sbuf_tensor(f"XB{i}", [128, NCH * T], F32)) for i in range(2)]
    NB = [st.enter_context(nc.sbuf_tensor(f"NB{i}", [128, NCH * T], BF16)) for i in range(2)]
    STT = [st.enter_context(nc.sbuf_tensor(f"ST{i}", [128, T], F32)) for i in range(2)]
    HR = st.enter_context(nc.sbuf_tensor("HR", [128, NCH * T], F32))
    SS = st.enter_context(nc.sbuf_tensor("SS", [128, 2 * 4 * 2 * 512], F32))
    CV = st.enter_context(nc.sbuf_tensor("CV", [128, CV_N], F32))
    CB = st.enter_context(nc.sbuf_tensor("CB", [128, 768], BF16))
    CFH = st.enter_context(nc.sbuf_tensor("CFH", [128, 8 * 32], BF16))
    SCH = st.enter_context(nc.sbuf_tensor("SCH", [128, 16], F32))
    QD = st.enter_context(nc.sbuf_tensor("QD", [128, 2048], BF16))
    DG = st.enter_context(nc.sbuf_tensor("DG", [128, 512], BF16))
    PS = st.enter_context(nc.psum_tensor("PS", [128, 8, 512], F32))

    ident_f = CV[:, CV_ID:CV_ID + 128]
    ident_b = CB[:, 0:128]
    ones_b = CB[:, 128:256]
    tri_b = CB[:, 256:768]

    def cvcol(c):
        return CV[:, c:c + 1]

    def abf(u, a=0, b=512):
        return AR[:, u * 512 + a:u * 512 + b]

    def af32(u, a=0, b=512):
        return AR[:, u * 512:(u + 2) * 512].bitcast(F32)[:, a:b]

    def ai32(u):
        return AR[:, u * 512:(u + 2) * 512].bitcast(I32)

    def ak(u, n=1):
        return [("A", u + i) for i in range(n)]

    bank_ctr = [0]

    reserved_banks = set()

    def nbank():
        while True:
            b = bank_ctr[0] % 8
            bank_ctr[0] += 1
            if b not in reserved_banks:
                return b

    def pk(b):
        return ("P", b)

    def mm_group(b, items, reads, a=0, bb=512):
        def fn(e, s):
            n = len(items)
            ins = None
            for i, (l, r) in enumerate(items):
                ins = e.matmul(PS[:, b, a:bb], lhsT=l, rhs=r, start=(i == 0), stop=(i == n - 1))
            return ins
        TR.add("pe", fn, reads=reads, writes=[pk(b)])

    def mm_multi(b, items, reads):
        def fn(e, s):
            n = len(items)
            ins = None
            for i, (o, l, r) in enumerate(items):
                ins = e.matmul(o, lhsT=l, rhs=r, start=(i == 0), stop=(i == n - 1))
            return ins
        TR.add("pe", fn, reads=reads, writes=[pk(b)])

    def act(out_, in_, func, reads, writes, bias=None, scale=None):
        kw = {}
        if bias is not None:
            kw["bias"] = bias
        if scale is not None:
            kw["scale"] = scale
        TR.add("act", lambda e, s: e.activation(out=out_, in_=in_, func=func, **kw), reads=reads, writes=writes)

    def vtt(eng, out_, in0, in1, op, reads, writes):
        TR.add(eng, lambda e, s: e.tensor_tensor(out=out_, in0=in0, in1=in1, op=op), reads=reads, writes=writes)

    def vts(eng, out_, in0, s1, s2, op0, op1, reads, writes):
        if s2 is None:
            TR.add(eng, lambda e, s: e.tensor_scalar(out=out_, in0=in0, scalar1=s1, scalar2=None, op0=op0),
                   reads=reads, writes=writes)
        else:
            TR.add(eng, lambda e, s: e.tensor_scalar(out=out_, in0=in0, scalar1=s1, scalar2=s2, op0=op0, op1=op1),
                   reads=reads, writes=writes)

    def vstt(out_, in0, scalar, in1, op0, op1, reads, writes):
        TR.add("dve", lambda e, s: e.scalar_tensor_tensor(out=out_, in0=in0, scalar=scalar, in1=in1, op0=op0, op1=op1),
               reads=reads, writes=writes)

    def vcopy(eng, out_, in_, reads, writes):
        TR.add(eng, lambda e, s: e.tensor_copy(out=out_, in_=in_), reads=reads, writes=writes)

    wctr = [0]

    def wload(key):
        pidx = pindex[key]
        slot = wctr[0] % NSLOT
        wctr[0] += 1
        ne = pieces[pidx][3] * pieces[pidx][5]
        TR.add("sp", lambda e, s: e.dma_start(out=WS[:, slot * 4096:slot * 4096 + ne], in_=wb[pidx][:, 0:ne]).then_inc(s, 16),
               reads=[("wb", pidx)], writes=[("W", slot)], dma=f"w{slot}")
        return slot

    def wap(slot, kc, ncols, c0, n=128):
        base = slot * 4096 + kc * ncols + c0
        return WS[:, base:base + n]

    def _cload(e, s):
        e.dma_start(out=CV[:, 0:CV_P0], in_=dram["cf32"][:, :]).then_inc(s, 16)
        e.dma_start(out=CV[:, CV_P0:P_END], in_=dram["pvec"][:, :]).then_inc(s, 16)
        e.dma_start(out=CB[:, :], in_=dram["cb16"][:, :]).then_inc(s, 16)
        e.dma_start(out=QD[:, :], in_=dram["qdec"][:, :]).then_inc(s, 16)
    TR.add("sp", _cload, writes=["C"], dma="c0", ndma=4)
    TR.add("pool", lambda e, s: e.memset(SS[:, :], 0.0), writes=[("S", r, h, dc) for r in range(2) for h in range(4) for dc in range(2)])
    TR.add("pool", lambda e, s: e.memset(CFH[:, :], 0.0), writes=[("CFH", c) for c in range(8)])
    TR.add("pool", lambda e, s: e.memset(SCH[:, :], 0.0), writes=[("SCH", c) for c in range(8)])
    vts("dve", CV[:, CV_HB:CV_HB + 8], CV[:, P_BPW1 + 8:P_BPW1 + 16], 0.5, None, ALU.mult, None, ["C"], ["C2"])

    cgrp = {"id": 0, "n": 0, "ops": [], "total": 0}

    def close_group():
        if cgrp["ops"]:
            last = cgrp["ops"][-1][1]
            for pi_, _ in cgrp["ops"]:
                TR.last_w[("wb", pi_)] = last
        cgrp["id"] += 1
        cgrp["n"] = 0
        cgrp["ops"] = []

    def cast_piece(pi):
        name, j, kc0, nkc, c0, ncols = pieces[pi]
        grp = cgrp["id"]
        src = dram[name][j, kc0 * 128:(kc0 + nkc) * 128, c0:c0 + ncols].rearrange("(k p) c -> p k c", p=128)
        dst = wb[pi][:, 0:nkc * ncols].rearrange("p (k c) -> p k c", c=ncols)
        rd = [("wb", cast_order[i]) for i in range(4)] if cgrp["total"] == 4 else []
        op = TR.add("pool", lambda e, s: e.dma_start(out=dst, in_=src).then_inc(s, 16), reads=rd, writes=[("wb", pi)], dma=f"cs{grp}")
        cgrp["ops"].append((pi, op))
        cgrp["n"] += 1
        cgrp["total"] += 1
        if cgrp["total"] <= 4 or cgrp["n"] >= 6:
            close_group()

    used_set = set()
    for k, pi in pindex.items():
        if k[0] in layers:
            used_set.add(pi)
    cast_order = [pi for pi in range(NP) if pi in used_set]
    cast_state = {"next": 0}

    def flush_casts(upto=None):
        n = len(cast_order) if upto is None else min(upto, len(cast_order))
        while cast_state["next"] < n:
            cast_piece(cast_order[cast_state["next"]])
            cast_state["next"] += 1
        close_group()

    store_ops = []

    def Xc(sl, c, a=0, b=T):
        return XB[sl][:, c * T + a:c * T + b]

    def Nc(sl, c, a=0, b=T):
        return NB[sl][:, c * T + a:c * T + b]

    def Hc(c, a=0, b=T):
        return HR[:, c * T + a:c * T + b]

    def STc(sl, i):
        return STT[sl][:, i * T:(i + 1) * T]

    def norm_g_col(L, n, c):
        return cvcol(P_NORM + (L * 4 + n) * 8 + c)

    def rms_stats(sl, src_ap, src_key):
        for c in range(NCH):
            act(Nc(sl, c), src_ap(c), AF.Square, [src_key(c)], [("N", sl, c)])
            if c % 2 == 1:
                yield None
        for _ in range(5):
            yield None
        b = nbank()
        mm_group(b, [(ones_b, Nc(sl, c)) for c in range(NCH)], ["C"] + [("N", sl, c) for c in range(NCH)])
        act(STc(sl, 0), PS[:, b, :], AF.Sqrt, [pk(b)], [("ST", sl, 0)], bias=RMS_EPS, scale=1.0 / D)
        for _ in range(3):
            yield None
        TR.add("dve", lambda e, s: e.reciprocal(out=STc(sl, 0), in_=STc(sl, 0)), reads=[("ST", sl, 0)], writes=[("ST", sl, 0)])
        for _ in range(3):
            yield None

    def pre_phase(sl, L, n):
        yield from rms_stats(sl, lambda c: Xc(sl, c), lambda c: ("X", sl, c))
        for c in range(NCH):
            vstt(Nc(sl, c), Xc(sl, c), norm_g_col(L, n, c), STc(sl, 0), ALU.mult, ALU.mult,
                 [("X", sl, c), ("ST", sl, 0), "C"], [("N", sl, c)])
            if c % 2 == 1:
                yield None

    def post_phase(sl, L, n):
        yield from rms_stats(sl, lambda c: Hc(c), lambda c: ("H", c))
        for c in range(NCH):
            vstt(Hc(c), Hc(c), norm_g_col(L, n, c), STc(sl, 0), ALU.mult, ALU.mult,
                 [("H", c), ("ST", sl, 0), "C"], [("H", c)])
            vtt("pool" if c % 2 else "dve", Xc(sl, c), Xc(sl, c), Hc(c), ALU.add, [("H", c), ("X", sl, c)], [("X", sl, c)])
            yield None

    U_STG = 0
    U_POS = 16

    def load_phase(sl, t):
        xk = [("X", sl, c) for c in range(NCH)]
        TR.add("pool", lambda e, s: e.dma_start(
            out=XB[sl][:, :].rearrange("p (j d) -> p j d", d=D),
            in_=dram["x"][t * T:(t + 1) * T, :].rearrange("(j p) d -> p j d", p=128)).then_inc(s, 16),
            writes=xk, dma=f"xl{sl}")
        for _ in range(14):
            yield None
        banks = [nbank() for _ in range(NCH)]
        for c in range(NCH):
            b = banks[c]

            def fn(e, s, b=b, c=c):
                ins = None
                for j in range(4):
                    ins = e.transpose(out=PS[:, b, j * 128:(j + 1) * 128], in_=XB[sl][:, j * D + c * 128:j * D + (c + 1) * 128], identity=ident_f)
                return ins
            TR.add("pe", fn, reads=xk + ["C"], writes=[pk(b)])
        for c in range(NCH):
            b = banks[c]
            if c % 2:
                act(Xc(sl, c), PS[:, b, :], AF.Copy, [pk(b)], [("X", sl, c)])
            else:
                vcopy("dve", Xc(sl, c), PS[:, b, :], [pk(b)], [("X", sl, c)])
        yield None

    U_CS = 70

    def cossin_phase(t):
        U_POS = 40
        TR.add("sp", lambda e, s: e.dma_start(out=ai32(U_POS), in_=dram["pos"][0:1, t * T:(t + 1) * T].partition_broadcast(128)).then_inc(s, 16),
               writes=ak(U_POS, 2), dma="pl")
        pf, ang, tq, kf, rr = (af32(U_POS + 2), af32(U_POS + 4), af32(U_POS + 6), af32(U_POS + 8), af32(U_POS + 10))
        ki = ai32(U_POS + 12)
        k_pf, k_ang, k_tq, k_kf, k_rr, k_ki = (ak(U_POS + 2, 2), ak(U_POS + 4, 2), ak(U_POS + 6, 2), ak(U_POS + 8, 2),
                                               ak(U_POS + 10, 2), ak(U_POS + 12, 2))
        vcopy("dve", pf, ai32(U_POS), ak(U_POS, 2), k_pf)
        vts("dve", ang, pf, cvcol(CV_INVF), None, ALU.mult, None, k_pf + ["C"], k_ang)
        for which in range(2):
            if which == 0:
                vts("dve", tq, ang, 1.0 / TWO_PI, None, ALU.mult, None, k_ang, k_tq)
            else:
                vts("dve", tq, ang, 1.0 / TWO_PI, 0.25, ALU.mult, ALU.add, k_ang, k_tq)
            vcopy("dve", ki, tq, k_tq, k_ki)
            vcopy("dve", kf, ki, k_ki, k_kf)
            vstt(rr, kf, -CW1, ang, ALU.mult, ALU.add, k_kf + k_ang, k_rr)
            vstt(rr, kf, -CW2, rr, ALU.mult, ALU.add, k_kf + k_rr, k_rr)
            if which == 1:
                vts("dve", rr, rr, math.pi / 2, None, ALU.add, None, k_rr, k_rr)
            vts("dve", rr, rr, -math.pi, math.pi, ALU.max, ALU.min, k_rr, k_rr)
            du = U_CS + 2 * (1 - which)
            act(af32(du), rr, AF.Sin, k_rr, ak(du, 2))
            yield None

    def store_phase(sl, t):
        xk = [("X", sl, c) for c in range(NCH)]
        banks = [nbank() for _ in range(NCH)]
        for j in range(4):
            for half in range(2):
                b = banks[j * 2 + half]

                def fn(e, s, b=b, j=j, half=half):
                    ins = None
                    for cc in range(4):
                        c = half * 4 + cc
                        ins = e.transpose(out=PS[:, b, cc * 128:(cc + 1) * 128], in_=Xc(sl, c, j * 128, (j + 1) * 128), identity=ident_f)
                    return ins
                TR.add("pe", fn, reads=xk + ["C"], writes=[pk(b)])
        for j in range(4):
            for half in range(2):
                b = banks[j * 2 + half]
                dst = XB[sl][:, j * D + half * 512:j * D + (half + 1) * 512]
                if half:
                    act(dst, PS[:, b, :], AF.Copy, [pk(b)], [("X", sl, j * 2 + half)])
                else:
                    vcopy("dve", dst, PS[:, b, :], [pk(b)], [("X", sl, j * 2 + half)])
        op = TR.add("pool", lambda e, s: e.dma_start(
            out=out[t * T:(t + 1) * T, :].rearrange("(j p) d -> p j d", p=128),
            in_=XB[sl][:, :].rearrange("p (j d) -> p j d", d=D)).then_inc(s, 16),
            reads=xk, dma="xs")
        store_ops.append(op)
        yield None

    U_H = 0
    U_R = 32

    def mlp_phase(sl, L):
        rctr = 0
        for p in range(8):
            slot = wload((L, "w1", p))
            for ocl in range(4):
                oc = p * 4 + ocl
                b = nbank()
                mm_group(b, [(wap(slot, kc, 512, ocl * 128), Nc(sl, kc)) for kc in range(NCH)],
                         [("W", slot)] + [("N", sl, kc) for kc in range(NCH)])
                ru = U_R + 2 * (rctr % 3)
                rctr += 1
                act(af32(ru), PS[:, b, :], AF.Relu, [pk(b)], ak(ru, 2))
                vtt("pool" if oc % 3 == 2 else "dve", abf(U_H + oc), af32(ru), af32(ru), ALU.mult, ak(ru, 2), ak(U_H + oc))
                yield None
        yield "sync"
        for dc in range(8):
            slot = wload((L, "w2", dc))
            b = nbank()
            mm_group(b, [(wap(slot, hc, 128, 0), abf(U_H + hc)) for hc in range(32)], [("W", slot)] + ak(U_H, 32))
            if dc % 2:
                act(Hc(dc), PS[:, b, :], AF.Copy, [pk(b)], [("H", dc)])
            else:
                vcopy("dve", Hc(dc), PS[:, b, :], [pk(b)], [("H", dc)])
            yield None

    U_SC_Y = 0
    U_SC_U = 8
    U_SC_T = 14
    U_SC_C = 18

    def sc_phase(sl, L):
        j = L // 3
        ctr = 0
        for cg in range(2):
            s_gc = wload((L, "gc", cg))
            s_hh = wload((L, "hh", cg))
            s_gb = wload((L, "gb", cg))
            for cl in range(4):
                c = cg * 4 + cl
                bq = {}
                for nm, slot in (("gc", s_gc), ("hh", s_hh), ("gb", s_gb)):
                    b = nbank()
                    bq[nm] = b
                    mm_group(b, [(wap(slot, kc, 512, cl * 128), Nc(sl, kc)) for kc in range(NCH)],
                             [("W", slot)] + [("N", sl, kc) for kc in range(NCH)])
                par = ctr % 2
                ctr += 1
                uu = U_SC_U + 3 * par
                tt = U_SC_T + 2 * par
                cc = U_SC_C + 2 * par
                Uf = AR[:, uu * 512:(uu + 3) * 512].bitcast(F32)
                ukeys = ak(uu, 3)
                act(af32(tt), PS[:, bq["hh"], :], AF.Copy, [pk(bq["hh"])], ak(tt, 2))
                vcopy("pool", Uf[:, 0:2], SCH[:, 2 * c:2 * c + 2], [("SCH", c)], ukeys)
                vtt("dve", Uf[:, 2:514], PS[:, bq["gc"], :], af32(tt), ALU.mult, [pk(bq["gc"])] + ak(tt, 2), ukeys)
                vcopy("pool", SCH[:, 2 * c:2 * c + 2], Uf[:, 512:514], ukeys, [("SCH", c)])
                w0, w1, w2 = (cvcol(P_SCW + k * 8 + c) for k in range(3))
                vts("dve", af32(cc), Uf[:, 0:512], w0, None, ALU.mult, None, ukeys + ["C"], ak(cc, 2))
                vstt(af32(cc), Uf[:, 1:513], w1, af32(cc), ALU.mult, ALU.add, ukeys + ak(cc, 2) + ["C"], ak(cc, 2))
                vstt(af32(cc), Uf[:, 2:514], w2, af32(cc), ALU.mult, ALU.add, ukeys + ak(cc, 2) + ["C"], ak(cc, 2))
                vtt("dve", abf(U_SC_Y + c), PS[:, bq["gb"], :], af32(cc), ALU.mult, [pk(bq["gb"])] + ak(cc, 2), ak(U_SC_Y + c))
                yield None
        yield "sync"
        for p in range(2):
            slot = wload((L, "o", p))
            for dcl in range(4):
                dc = p * 4 + dcl
                b = nbank()
                mm_group(b, [(wap(slot, kc, 512, dcl * 128), abf(U_SC_Y + kc)) for kc in range(NCH)],
                         [("W", slot)] + ak(U_SC_Y, 8))
                if dc % 2:
                    act(Hc(dc), PS[:, b, :], AF.Copy, [pk(b)], [("H", dc)])
                else:
                    vcopy("dve", Hc(dc), PS[:, b, :], [pk(b)], [("H", dc)])
                yield None

    U_CF_G = 0
    U_CF_HC = 16
    U_CF_HB = 32
    U_CF_HQ = 40
    U_CF_HS = 48
    U_CF_T = 56
    U_CF_D = 68

    def cf_phase(sl, L):
        ctr = 0

        def Gap(c, a, b):
            return AR[:, (U_CF_G + 2 * c) * 512 + a:(U_CF_G + 2 * c) * 512 + b]
        for cg in range(2):
            s_a = wload((L, "a", cg))
            s_g = wload((L, "gt", cg))
            for cl in range(4):
                c = cg * 4 + cl
                ba = nbank()
                mm_group(ba, [(wap(s_a, kc, 512, cl * 128), Nc(sl, kc)) for kc in range(NCH)],
                         [("W", s_a)] + [("N", sl, kc) for kc in range(NCH)])
                bg = nbank()
                mm_group(bg, [(wap(s_g, kc, 512, cl * 128), Nc(sl, kc)) for kc in range(NCH)],
                         [("W", s_g)] + [("N", sl, kc) for kc in range(NCH)])
                tu = U_CF_T + 2 * (ctr % 2)
                ctr += 1
                act(af32(tu), PS[:, bg, :], AF.Tanh, [pk(bg), "C2"], ak(tu, 2), bias=cvcol(CV_HB + c), scale=0.5)
                vts("pool", af32(tu), af32(tu), 0.5, 0.5, ALU.mult, ALU.add, ak(tu, 2), ak(tu, 2))
                gk = ak(U_CF_G + 2 * c, 2)
                vcopy("pool", Gap(c, 0, 30), CFH[:, c * 32:c * 32 + 30], [("CFH", c)], gk)
                vstt(Gap(c, 30, 542), PS[:, ba, :], cvcol(P_BPW1 + c), af32(tu), ALU.add, ALU.mult,
                     [pk(ba), "C"] + ak(tu, 2), gk)
                vcopy("pool", CFH[:, c * 32:c * 32 + 30], Gap(c, 512, 542), gk, [("CFH", c)])
                yield None
        dctr = 0
        for c in range(NCH):
            b = nbank()
            gk = ak(U_CF_G + 2 * c, 2)
            for k in range(31):
                ds = dctr % 4
                dctr += 1
                dap = DG[:, ds * 128:(ds + 1) * 128]
                dkey = ("DG", ds)
                vts("pool", dap, ident_b, cvcol(P_WDW + k * 8 + c), 0.0, ALU.mult, ALU.add, ["C"], [dkey])
                TR.add("pe", lambda e, s, b=b, dap=dap, c=c, k=k: e.matmul(PS[:, b, :], lhsT=dap, rhs=Gap(c, k, k + 512),
                                                                            start=(k == 0), stop=(k == 30)),
                       reads=[dkey] + gk, writes=[pk(b)])
                if k % 4 == 3:
                    yield None
            act(af32(U_CF_HC + 2 * c), PS[:, b, :], AF.Identity, [pk(b), "C"], ak(U_CF_HC + 2 * c, 2), bias=cvcol(P_BDW + c))
            act(abf(U_CF_HB + c), PS[:, b, :], AF.Identity, [pk(b), "C"], ak(U_CF_HB + c), bias=cvcol(P_BDW + c))
            act(abf(U_CF_HQ + c), PS[:, b, :], AF.Square, [pk(b), "C"], ak(U_CF_HQ + c), bias=cvcol(P_BDW + c))
            yield None
        b1 = nbank()
        mm_group(b1, [(ones_b, abf(U_CF_HB + c)) for c in range(NCH)], ["C"] + ak(U_CF_HB, 8))
        b2 = nbank()
        mm_group(b2, [(ones_b, abf(U_CF_HQ + c)) for c in range(NCH)], ["C"] + ak(U_CF_HQ, 8))
        mean, msq, rstd, bm = (U_CF_T + 4, U_CF_T + 6, U_CF_T + 8, U_CF_T + 10)
        act(af32(mean), PS[:, b1, :], AF.Copy, [pk(b1)], ak(mean, 2), scale=1.0 / D)
        vtt("dve", af32(msq), af32(mean), af32(mean), ALU.mult, ak(mean, 2), ak(msq, 2))
        vstt(af32(msq), PS[:, b2, :], 1.0 / D, af32(msq), ALU.mult, ALU.subtract, [pk(b2)] + ak(msq, 2), ak(msq, 2))
        act(af32(rstd), af32(msq), AF.Sqrt, ak(msq, 2), ak(rstd, 2), bias=LN_EPS)
        TR.add("dve", lambda e, s: e.reciprocal(out=af32(rstd), in_=af32(rstd)), reads=ak(rstd, 2), writes=ak(rstd, 2))
        vstt(af32(bm), af32(mean), -1.0, af32(rstd), ALU.mult, ALU.mult, ak(mean, 2) + ak(rstd, 2), ak(bm, 2))
        yield None
        for c in range(NCH):
            hk = ak(U_CF_HC + 2 * c, 2)
            eng = "pool" if c % 2 else "dve"
            vtt(eng, af32(U_CF_HC + 2 * c), af32(U_CF_HC + 2 * c), af32(rstd), ALU.mult, hk + ak(rstd, 2), hk)
            vtt(eng, af32(U_CF_HC + 2 * c), af32(U_CF_HC + 2 * c), af32(bm), ALU.add, hk + ak(bm, 2), hk)
            act(abf(U_CF_HS + c), af32(U_CF_HC + 2 * c), AF.Silu, hk + ["C"], ak(U_CF_HS + c),
                bias=cvcol(P_LNB + c), scale=cvcol(P_LNG + c))
            yield None
        yield "sync"
        for p in range(2):
            slot = wload((L, "o", p))
            for dcl in range(4):
                dc = p * 4 + dcl
                b = nbank()
                mm_group(b, [(wap(slot, kc, 512, dcl * 128), abf(U_CF_HS + kc)) for kc in range(NCH)],
                         [("W", slot)] + ak(U_CF_HS, 8))
                act(Hc(dc), PS[:, b, :], AF.Identity, [pk(b), "C"], [("H", dc)], bias=cvcol(P_BPW2 + dc))
                yield None

    U_RT_SET = 0
    U_RT_KT = 24
    U_RT_SC = 26
    U_RT_Y = 30
    U_RT_YQ = 34
    U_RT_SB = 38
    U_RT_YG = 40
    U_RT_F = 56

    def ret_phase(sl, L):
        r = L // 3
        cos_t = af32(U_CS)
        sin_t = af32(U_CS + 2)
        ck = ak(U_CS, 4)
        fa, fb, fcq, fsq = U_RT_F, U_RT_F + 2, U_RT_F + 4, U_RT_F + 6
        mean, msq, rstd = U_RT_F + 8, U_RT_F + 10, U_RT_F + 12
        bm = msq
        xnk = [("N", sl, kc) for kc in range(NCH)]

        def sbase(h):
            return U_RT_SET + 12 * (h % 2)

        def Sap(h, dc):
            o = ((r * 4 + h) * 2 + dc) * 512
            return SS[:, o:o + 512]

        def rotary(bt1, bt2, ta, tb, out1, out2, ok1, ok2):
            vtt("dve", af32(ta), PS[:, bt1, :], cos_t, ALU.mult, [pk(bt1)] + ck, ak(ta, 2))
            vtt("dve", af32(tb), PS[:, bt2, :], sin_t, ALU.mult, [pk(bt2)] + ck, ak(tb, 2))
            vtt("pool", out1, af32(ta), af32(tb), ALU.subtract, ak(ta, 2) + ak(tb, 2), ok1)
            vtt("dve", af32(ta), PS[:, bt2, :], cos_t, ALU.mult, [pk(bt2)] + ck, ak(ta, 2))
            vtt("dve", af32(tb), PS[:, bt1, :], sin_t, ALU.mult, [pk(bt1)] + ck, ak(tb, 2))
            vtt("pool", out2, af32(ta), af32(tb), ALU.add, ak(ta, 2) + ak(tb, 2), ok2)

        slots = {}

        def proj(h):
            sb = sbase(h)
            qd, kT, v, sg = sb, sb + 2, sb + 4, sb + 8
            slots["q"] = wload((L, "q", h))
            slots["k"] = wload((L, "k", h))
            for nm, dstu, ta, tb in (("q", qd, fa, fb), ("k", kT, fcq, fsq)):
                slot = slots[nm]
                b1 = nbank()
                mm_group(b1, [(wap(slot, kc, 256, 0), Nc(sl, kc)) for kc in range(NCH)], [("W", slot)] + xnk)
                b2 = nbank()
                mm_group(b2, [(wap(slot, kc, 256, 128), Nc(sl, kc)) for kc in range(NCH)], [("W", slot)] + xnk)
                rotary(b1, b2, ta, tb, abf(dstu), abf(dstu + 1), ak(dstu), ak(dstu + 1))
                yield None
            slot = wload((L, "v", h))
            for jc in range(4):
                b = nbank()
                mm_group(b, [(Nc(sl, kc, jc * 128, (jc + 1) * 128), wap(slot, kc, 512, 0, 512)) for kc in range(NCH)],
                         [("W", slot)] + xnk)
                act(abf(v + jc), PS[:, b, :], AF.Copy, [pk(b)], ak(v + jc))
                yield None
            slot = wload((L, "g", h))
            for cl in range(4):
                b = nbank()
                mm_group(b, [(wap(slot, kc, 512, cl * 128), Nc(sl, kc)) for kc in range(NCH)], [("W", slot)] + xnk)
                act(abf(sg + cl), PS[:, b, :], AF.Silu, [pk(b)], ak(sg + cl))
                yield None

        def attn(h):
            sb = sbase(h)
            qd, kT, v, sg = sb, sb + 2, sb + 4, sb + 8
            for jc in range(4):
                n = 512 - jc * 128
                b = nbank()
                mm_group(b, [(abf(kT + dc, jc * 128, (jc + 1) * 128), abf(qd + dc, jc * 128, 512)) for dc in range(2)],
                         ak(kT, 2) + ak(qd, 2), 0, n)
                vstt(abf(U_RT_SC + jc, 0, n), PS[:, b, 0:n], cvcol(CV_KINV + jc * 4 + h), tri_b[:, 0:n], ALU.mult, ALU.mult,
                     [pk(b), "C"], ak(U_RT_SC + jc))
            yield None
            b = nbank()
            pb = PS[:, b, :].bitcast(BF16)

            def fn(e, s):
                ins = None
                for jc in range(4):
                    for dc in range(2):
                        ins = e.transpose(out=pb[:, jc * 256 + dc * 128:jc * 256 + (dc + 1) * 128],
                                          in_=abf(kT + dc, jc * 128, (jc + 1) * 128), identity=ident_b)
                return ins
            TR.add("pe", fn, reads=ak(kT, 2) + ["C"], writes=[pk(b)])
            for jc in range(4):
                act(AR[:, U_RT_KT * 512 + jc * 256:U_RT_KT * 512 + (jc + 1) * 256], pb[:, jc * 256:(jc + 1) * 256], AF.Copy,
                    [pk(b), "C"], ak(U_RT_KT, 2), scale=cvcol(CV_KDEC + jc * 4 + h))
            yield None
            for dc in range(2):
                act(abf(U_RT_SB + dc), Sap(h, dc), AF.Copy, [("S", r, h, dc)], ak(U_RT_SB + dc))
            for dvc in range(4):
                b = nbank()
                items = []
                for dc in range(2):
                    items.append((PS[:, b, :], abf(U_RT_SB + dc, dvc * 128, (dvc + 1) * 128), abf(qd + dc)))
                for jc in range(4):
                    items.append((PS[:, b, jc * 128:512], abf(v + jc, dvc * 128, (dvc + 1) * 128), abf(U_RT_SC + jc, 0, 512 - jc * 128)))
                mm_multi(b, items, ak(U_RT_SB, 2) + ak(qd, 2) + ak(v, 4) + ak(U_RT_SC, 4))
                vtt("dve", abf(U_RT_Y + dvc), PS[:, b, :], QD[:, h * 512:(h + 1) * 512], ALU.mult, [pk(b), "C"], ak(U_RT_Y + dvc))
                act(abf(U_RT_YQ + dvc), abf(U_RT_Y + dvc), AF.Square, ak(U_RT_Y + dvc), ak(U_RT_YQ + dvc))
                yield None
            for dc in range(2):
                b = nbank()
                mm_group(b, [(AR[:, U_RT_KT * 512 + jc * 256 + dc * 128:U_RT_KT * 512 + jc * 256 + (dc + 1) * 128], abf(v + jc))
                             for jc in range(4)], ak(U_RT_KT, 2) + ak(v, 4))
                vstt(Sap(h, dc), Sap(h, dc), g512[h], PS[:, b, :], ALU.mult, ALU.add, [pk(b), ("S", r, h, dc)], [("S", r, h, dc)])
            yield None
            b1 = nbank()
            mm_group(b1, [(ones_b, abf(U_RT_Y + c)) for c in range(4)], ["C"] + ak(U_RT_Y, 4))
            b2 = nbank()
            mm_group(b2, [(ones_b, abf(U_RT_YQ + c)) for c in range(4)], ["C"] + ak(U_RT_YQ, 4))
            act(af32(mean), PS[:, b1, :], AF.Copy, [pk(b1)], ak(mean, 2), scale=1.0 / 512)
            vtt("dve", af32(msq), af32(mean), af32(mean), ALU.mult, ak(mean, 2), ak(msq, 2))
            vstt(af32(msq), PS[:, b2, :], 1.0 / 512, af32(msq), ALU.mult, ALU.subtract, [pk(b2)] + ak(msq, 2), ak(msq, 2))
            act(af32(rstd), af32(msq), AF.Sqrt, ak(msq, 2), ak(rstd, 2), bias=LN_EPS)
            TR.add("dve", lambda e, s: e.reciprocal(out=af32(rstd), in_=af32(rstd)), reads=ak(rstd, 2), writes=ak(rstd, 2))
            vstt(af32(bm), af32(mean), -1.0, af32(rstd), ALU.mult, ALU.mult, ak(mean, 2) + ak(rstd, 2), ak(bm, 2))
            yield None
            for c in range(4):
                tmp = fa if c % 2 == 0 else fb
                vtt("pool", af32(tmp), abf(U_RT_Y + c), af32(rstd), ALU.mult, ak(U_RT_Y + c) + ak(rstd, 2), ak(tmp, 2))
                vtt("dve", af32(tmp), af32(tmp), af32(bm), ALU.add, ak(tmp, 2) + ak(bm, 2), ak(tmp, 2))
                vstt(abf(U_RT_YG + h * 4 + c), af32(tmp), cvcol(P_GN + r * 16 + h * 4 + c), abf(sg + c), ALU.mult, ALU.mult,
                     ak(tmp, 2) + ak(sg + c) + ["C"], ak(U_RT_YG + h * 4 + c))
            yield None

        yield from cossin_phase(tile_of_slot[sl])
        yield from proj(0)
        carry = None
        for h in range(4):
            ga = attn(h)
            gp = proj(h + 1) if h + 1 < 4 else iter(())
            alive = {"A": True, "P": h + 1 < 4, "C": carry is not None}
            gens_ = {"A": ga, "P": gp, "C": carry}
            na = 0
            for ch in "PPAPCPAPPAAPAAPAPAP":
                if not alive[ch]:
                    continue
                if ch == "A":
                    if na >= 8:
                        continue
                    na += 1
                try:
                    next(gens_[ch])
                except StopIteration:
                    alive[ch] = False
                    continue
                yield None
            for ch in "CP":
                while alive[ch]:
                    try:
                        next(gens_[ch])
                    except StopIteration:
                        alive[ch] = False
                        break
                    yield None
            while na < 8:
                next(ga)
                na += 1
                yield None
            carry = ga
        early = {}
        oslots = {}
        for p in range(2):
            oslots[p] = wload((L, "o", p))
            for dcl in range(2):
                dc = p * 2 + dcl
                b = nbank()
                reserved_banks.add(b)
                early[dc] = b

                def fnp(e, s, b=b, slot=oslots[p], dcl=dcl):
                    ins = None
                    for kc in range(12):
                        ins = e.matmul(PS[:, b, :], lhsT=wap(slot, kc, 256, dcl * 128), rhs=abf(U_RT_YG + kc), start=(kc == 0), stop=False)
                    return ins
                TR.add("pe", fnp, reads=[("W", oslots[p])] + ak(U_RT_YG, 12), writes=[pk(b)])
            yield None
        for _ in carry:
            yield None
        yield "sync"
        for p in range(4):
            slot = oslots[p] if p < 2 else wload((L, "o", p))
            for dcl in range(2):
                dc = p * 2 + dcl
                if dc in early:
                    b = early[dc]

                    def fnf(e, s, b=b, slot=slot, dcl=dcl):
                        ins = None
                        for kc in range(12, 16):
                            ins = e.matmul(PS[:, b, :], lhsT=wap(slot, kc, 256, dcl * 128), rhs=abf(U_RT_YG + kc), start=False, stop=(kc == 15))
                        return ins
                    TR.add("pe", fnf, reads=[("W", slot)] + ak(U_RT_YG + 12, 4), writes=[pk(b)])
                    reserved_banks.discard(b)
                else:
                    b = nbank()
                    mm_group(b, [(wap(slot, kc, 256, dcl * 128), abf(U_RT_YG + kc)) for kc in range(16)],
                             [("W", slot)] + ak(U_RT_YG, 16))
                if dc % 2:
                    act(Hc(dc), PS[:, b, :], AF.Copy, [pk(b)], [("H", dc)])
                else:
                    vcopy("dve", Hc(dc), PS[:, b, :], [pk(b)], [("H", dc)])
                yield None

    def mixer_phase(sl, L):
        kind = L % 3
        if kind == 0:
            yield from ret_phase(sl, L)
        elif kind == 1:
            yield from cf_phase(sl, L)
        else:
            yield from sc_phase(sl, L)

    tile_of_slot = [0, 0]

    def thread(sl, t):
        tile_of_slot[sl] = t
        yield from load_phase(sl, t)
        for L in layers:
            yield from pre_phase(sl, L, 0)
            yield ("acq", ("mm", L, 0))
            if t == 0:
                flush_casts(upto=phase_end[(L, 1)])
            yield from mixer_phase(sl, L)
            yield ("rel", ("mm", L, 0))
            yield from post_phase(sl, L, 1)
            yield from pre_phase(sl, L, 2)
            yield ("acq", ("mm", L, 1))
            if t == 0:
                nxt = phase_list.index((L, 1)) + 1
                if nxt < len(phase_list):
                    flush_casts(upto=phase_end[phase_list[nxt]])
            yield from mlp_phase(sl, L)
            yield ("rel", ("mm", L, 1))
            yield from post_phase(sl, L, 3)
        yield from store_phase(sl, t)

    phase_end = {}
    cnt = 0
    for L_ in layers:
        nmix = sum(1 for k in pindex if k[0] == L_ and k[1] not in ("w1", "w2"))
        cnt += nmix
        phase_end[(L_, 0)] = cnt
        cnt += 16
        phase_end[(L_, 1)] = cnt
    phase_list = [(L_, m_) for L_ in layers for m_ in (0, 1)]
    flush_casts(upto=4)
    first_ops = [TR.last_w[("wb", cast_order[i])] for i in range(min(4, len(cast_order)))]
    flush_casts(upto=phase_end[phase_list[0]])

    done = {}
    gens = [None, None]
    tiles_of = [None, None]
    pending = [None, None]
    next_tile = 0
    owner = None

    def advance(sl):
        nonlocal owner
        g = gens[sl]
        if g is None:
            return False
        t = tiles_of[sl]
        if pending[sl] is not None:
            pid = pending[sl]
            if owner is not None or (t > 0 and not done.get((t - 1, pid))):
                return False
            owner = sl
            pending[sl] = None
        try:
            v = next(g)
        except StopIteration:
            gens[sl] = None
            return False
        if isinstance(v, tuple):
            if v[0] == "acq":
                pending[sl] = v[1]
            else:
                done[(t, v[1])] = True
                owner = None
        elif v == "sync":
            o = 1 - sl
            while gens[o] is not None and pending[o] is None:
                if not advance(o):
                    break
        return True

    while True:
        for sl in range(2):
            if gens[sl] is None and next_tile < ntiles:
                gens[sl] = thread(sl, next_tile)
                tiles_of[sl] = next_tile
                pending[sl] = None
                next_tile += 1
        if gens[0] is None and gens[1] is None:
            break
        order = [owner, 1 - owner] if owner is not None else [0, 1]
        if owner is None and gens[0] is not None and gens[1] is not None and tiles_of[1] < tiles_of[0]:
            order = [1, 0]
        prog = False
        for oi, sl in enumerate(order):
            if advance(sl):
                prog = True
            if oi == 1 and owner is not None and owner != sl:
                for _ in range(1):
                    if advance(sl):
                        prog = True
        if not prog and (gens[0] is not None or gens[1] is not None):
            raise RuntimeError(f"scheduler deadlock owner={owner} pending={pending} tiles={tiles_of} gens={[g is not None for g in gens]} done={sorted(done, key=str)[-6:]}")

    flush_casts()
    nsig = TR.emit(nc, st, final_waits=store_ops)
    st.close()
    return nc, nsig, len(TR.ops)


_CACHE = {}


def kernel(x, positions, norm_g, ret_w_in, ret_gn_g, ret_w_out, cf_w_pw1, cf_b_pw1, cf_w_dw, cf_b_dw,
           cf_ln_g, cf_ln_b, cf_w_pw2, cf_b_pw2, sc_w_in, sc_w_conv, sc_w_out, mlp_w1, mlp_w2):
    inp = dict(norm_g=norm_g, ret_gn_g=ret_gn_g, cf_b_pw1=cf_b_pw1, cf_b_dw=cf_b_dw, cf_ln_g=cf_ln_g,
               cf_ln_b=cf_ln_b, cf_b_pw2=cf_b_pw2, cf_w_dw=cf_w_dw, sc_w_conv=sc_w_conv)
    inp = {k: np.asarray(v) for k, v in inp.items()}
    cf32, qdec, g512, cb16 = host_consts()
    pvec = pack_params(inp)
    if "nc" not in _CACHE:
        _CACHE["nc"] = build_program(ntiles=S // T, layers=(0, 1, 2, 3), g512=g512)[0]
    nc = _CACHE["nc"]
    x = np.asarray(x, np.float32)
    positions = np.asarray(positions, np.int32)
    B = x.shape[0]
    shared = {
        "cf32": cf32, "pvec": pvec, "cb16": cb16, "qdec": qdec,
        "ret_w_in": np.ascontiguousarray(ret_w_in, np.float32), "ret_w_out": np.ascontiguousarray(ret_w_out, np.float32),
        "cf_w_pw1": np.ascontiguousarray(cf_w_pw1, np.float32), "cf_w_pw2": np.ascontiguousarray(cf_w_pw2, np.float32),
        "sc_w_in": np.ascontiguousarray(sc_w_in, np.float32), "sc_w_out": np.ascontiguousarray(sc_w_out, np.float32),
        "mlp_w1": np.ascontiguousarray(mlp_w1, np.float32), "mlp_w2": np.ascontiguousarray(mlp_w2, np.float32),
    }
    in_maps = []
    for b in range(B):
        m = dict(shared)
        m["x"] = np.ascontiguousarray(x[b])
        m["pos"] = np.ascontiguousarray(positions[b][None, :])
        in_maps.append(m)
    res = run_bass_kernel_spmd(nc, in_maps, core_ids=list(range(B)))
    return np.stack([np.asarray(r["out"], np.float32) for r in res.results], axis=0)
```
